# Optimizing a Trainium2 kernel written in Bass

```python
import jax, jax.numpy as jnp
from jax import lax
import numpy as np

D_MODEL = 1024
BATCH = 8
SEQ = 4096
DEPTH = 4

D_MIX = D_MODEL
HEAD_DIM = 64
D_ATTN = D_MIX // 2
N_HEADS = D_ATTN // HEAD_DIM
D_CONV = D_MIX - D_ATTN
D_IN = 3 * D_ATTN + 2 * D_CONV
CONV_WIDTH = 31
DILATED_BRANCHES = ((128, 1), (512, 4), (2048, 16))
ATTN_BLOCK = 128
N_GROUPS = 4
EXPERTS_PER_GROUP = 8
N_EXPERTS = N_GROUPS * EXPERTS_PER_GROUP
TOP_K_INNER = 2
D_EXPERT = D_MODEL // 2
MOE_BLOCK = 256
NORM_EPS = 1e-6
N_MOD = 6

kernel_name = "hybrid_dilated_attn_conformer_hmoe"


def rms_norm(x, g):
    x32 = x.astype(jnp.float32)
    y = x32 * lax.rsqrt(jnp.mean(x32 * x32, axis=-1, keepdims=True) + NORM_EPS)
    return (y * g.astype(jnp.float32)).astype(x.dtype)


def layer_norm(x, g, b):
    x32 = x.astype(jnp.float32)
    mu = jnp.mean(x32, axis=-1, keepdims=True)
    var = jnp.mean(jnp.square(x32 - mu), axis=-1, keepdims=True)
    y = (x32 - mu) * lax.rsqrt(var + NORM_EPS)
    return (y * g.astype(jnp.float32) + b.astype(jnp.float32)).astype(x.dtype)


def alibi_slopes(n_heads):
    return jnp.asarray(2.0 ** (-8.0 * np.arange(1, n_heads + 1) / n_heads), dtype=jnp.float32)


def dilated_branch(q, k, v, slopes, window, dil):
    B, H, S, Dh = q.shape
    L = S // dil
    n_back = window // dil
    nb = -(-L // ATTN_BLOCK)
    Lp = nb * ATTN_BLOCK

    def to_blocks(t):
        t = t.reshape(B, H, L, dil, Dh).transpose(0, 1, 3, 2, 4)
        t = jnp.pad(t, ((0, 0), (0, 0), (0, 0), (0, Lp - L), (0, 0)))
        return t.reshape(B, H, dil, nb, ATTN_BLOCK, Dh)

    def with_prev(t):
        prev = jnp.pad(t[:, :, :, :-1], ((0, 0), (0, 0), (0, 0), (1, 0), (0, 0), (0, 0)))
        return jnp.concatenate([prev, t], axis=4)

    qb = to_blocks(q)
    kc = with_prev(to_blocks(k))
    vc = with_prev(to_blocks(v))
    s = jnp.einsum('bhrnqd,bhrnkd->bhrnqk', qb, kc,
                   preferred_element_type=jnp.float32) * (HEAD_DIM ** -0.5)
    qi = np.arange(ATTN_BLOCK)[:, None] + ATTN_BLOCK
    ki = np.arange(2 * ATTN_BLOCK)[None, :]
    delta = qi - ki
    blk_ok = (np.arange(nb)[:, None, None] > 0) | (ki[None] >= ATTN_BLOCK)
    valid = (delta >= 0) & (delta <= n_back) & blk_ok
    bias = -slopes[:, None, None] * jnp.asarray(delta * dil, dtype=jnp.float32)
    s = s + bias[None, :, None, None]
    s = jnp.where(valid[None, None, None], s, -jnp.inf)
    m = jnp.max(s, axis=-1)
    p = jnp.exp(s - m[..., None])
    l = jnp.sum(p, axis=-1)
    o = jnp.einsum('bhrnqk,bhrnkd->bhrnqd', p, vc.astype(jnp.float32))

    def from_blocks(t, trailing):
        t = t.reshape((B, H, dil, Lp) + trailing)[:, :, :, :L]
        t = jnp.moveaxis(t, 2, 3)
        return t.reshape((B, H, S) + trailing)

    return from_blocks(o, (Dh,)), from_blocks(m, ()), from_blocks(l, ())


def dilated_attention(q, k, v, slopes):
    outs = [dilated_branch(q, k, v, slopes, w, d) for (w, d) in DILATED_BRANCHES]
    m_all = jnp.stack([m for (_, m, _) in outs])
    m_max = jnp.max(m_all, axis=0)
    num = 0.0
    den = 0.0
    for (o, m, l) in outs:
        a = jnp.exp(m - m_max)
        num = num + a[..., None] * o
        den = den + a * l
    return num / den[..., None]


def conformer_conv(a, b, conv_w, conv_b, ln_g, ln_b):
    u = a * jax.nn.sigmoid(b)
    y = lax.conv_general_dilated(u, conv_w[:, None, :].astype(u.dtype), window_strides=(1,),
                                 padding=[(CONV_WIDTH - 1, 0)],
                                 dimension_numbers=('NWC', 'WIO', 'NWC'),
                                 feature_group_count=D_CONV) + conv_b
    return jax.nn.silu(layer_norm(y, ln_g, ln_b))


def hier_moe(h, w_rg, b_rg, w_re, b_re, w_gate, w_up, w_down):
    B, S, D = h.shape
    T = B * S
    hf = h.reshape(T, D)
    tix = jnp.arange(T)
    g_logits = (hf @ w_rg + b_rg).astype(jnp.float32)
    g_prob = jax.nn.softmax(g_logits, axis=-1)
    grp = jnp.argmax(g_logits, axis=-1)
    p_grp = g_prob[tix, grp]
    e_logits = (hf @ w_re + b_re).astype(jnp.float32).reshape(T, N_GROUPS, EXPERTS_PER_GROUP)
    e_logits = e_logits[tix, grp]
    top_val, top_idx = lax.top_k(e_logits, TOP_K_INNER)
    top_w = jax.nn.softmax(top_val, axis=-1) * p_grp[:, None]

    A = T * TOP_K_INNER
    eid = (grp[:, None] * EXPERTS_PER_GROUP + top_idx).reshape(A).astype(jnp.int32)
    tok = jnp.repeat(tix, TOP_K_INNER).astype(jnp.int32)
    wts = top_w.reshape(A)
    order = jnp.argsort(eid)
    s_eid, s_tok, s_w = eid[order], tok[order], wts[order]
    counts = jnp.bincount(eid, length=N_EXPERTS)
    start = jnp.cumsum(counts) - counts
    padded = ((counts + MOE_BLOCK - 1) // MOE_BLOCK) * MOE_BLOCK
    pad_end = jnp.cumsum(padded)
    pad_start = pad_end - padded
    dest = pad_start[s_eid] + (jnp.arange(A) - start[s_eid])
    P = (-(-A // MOE_BLOCK)) * MOE_BLOCK + N_EXPERTS * MOE_BLOCK
    nblk = P // MOE_BLOCK
    buf_tok = jnp.full((P,), T, dtype=jnp.int32).at[dest].set(s_tok)
    buf_w = jnp.zeros((P,), dtype=jnp.float32).at[dest].set(s_w)
    blk_eid = jnp.clip(jnp.searchsorted(pad_end, jnp.arange(nblk) * MOE_BLOCK, side='right'),
                       0, N_EXPERTS - 1)
    x_pad = jnp.concatenate([hf, jnp.zeros((1, D), hf.dtype)], axis=0)
    xb = x_pad[buf_tok].reshape(nblk, MOE_BLOCK, D)

    def expert_block(args):
        xblk, e = args
        return (jax.nn.silu(xblk @ w_gate[e]) * (xblk @ w_up[e])) @ w_down[e]

    yb = lax.map(expert_block, (xb, blk_eid)).reshape(P, D)
    out = jnp.zeros((T + 1, D), yb.dtype).at[buf_tok].add(yb * buf_w[:, None].astype(yb.dtype))[:T]
    return out.reshape(B, S, D).astype(h.dtype)


def setup_inputs(seed: int = 0) -> dict:
    key = jax.random.key(seed)
    ks = jax.random.split(key, 24)
    f32 = jnp.float32
    nrm = lambda k, shape, s: jax.random.normal(k, shape, f32) * s
    L = DEPTH
    return {
        "x": nrm(ks[0], (BATCH, SEQ, D_MODEL), 1.0),
        "c": nrm(ks[1], (BATCH, D_MODEL), 1.0),
        "w_mod": nrm(ks[2], (L, D_MODEL, N_MOD * D_MODEL), 0.5 * D_MODEL ** -0.5),
        "b_mod": nrm(ks[3], (L, N_MOD * D_MODEL), 0.01),
        "g_norm1": 1.0 + nrm(ks[4], (L, D_MODEL), 0.02),
        "w_in": nrm(ks[5], (L, D_MODEL, D_IN), D_MODEL ** -0.5),
        "conv_w": nrm(ks[6], (L, CONV_WIDTH, D_CONV), CONV_WIDTH ** -0.5),
        "conv_b": nrm(ks[7], (L, D_CONV), 0.01),
        "conv_ln_g": 1.0 + nrm(ks[8], (L, D_CONV), 0.02),
        "conv_ln_b": nrm(ks[9], (L, D_CONV), 0.01),
        "w_out": nrm(ks[10], (L, D_MIX, D_MODEL), D_MIX ** -0.5),
        "g_norm2": 1.0 + nrm(ks[11], (L, D_MODEL), 0.02),
        "w_router_group": nrm(ks[12], (L, D_MODEL, N_GROUPS), D_MODEL ** -0.5),
        "b_router_group": nrm(ks[13], (L, N_GROUPS), 0.01),
        "w_router_expert": nrm(ks[14], (L, D_MODEL, N_EXPERTS), D_MODEL ** -0.5),
        "b_router_expert": nrm(ks[15], (L, N_EXPERTS), 0.01),
        "w_exp_gate": nrm(ks[16], (L, N_EXPERTS, D_MODEL, D_EXPERT), D_MODEL ** -0.5),
        "w_exp_up": nrm(ks[17], (L, N_EXPERTS, D_MODEL, D_EXPERT), D_MODEL ** -0.5),
        "w_exp_down": nrm(ks[18], (L, N_EXPERTS, D_EXPERT, D_MODEL), D_EXPERT ** -0.5),
        "g_final": 1.0 + nrm(ks[19], (D_MODEL,), 0.02),
    }


def reference(x, c, w_mod, b_mod, g_norm1, w_in, conv_w, conv_b, conv_ln_g, conv_ln_b, w_out,
              g_norm2, w_router_group, b_router_group, w_router_expert, b_router_expert,
              w_exp_gate, w_exp_up, w_exp_down, g_final):
    B, S, D = x.shape
    slopes = alibi_slopes(N_HEADS)
    c_act = jax.nn.silu(c)

    def split_heads(t):
        return t.reshape(B, S, N_HEADS, HEAD_DIM).transpose(0, 2, 1, 3)

    for l in range(DEPTH):
        mod = c_act @ w_mod[l] + b_mod[l]
        sh1, sc1, gt1, sh2, sc2, gt2 = jnp.split(mod[:, None, :], N_MOD, axis=-1)

        h = rms_norm(x, g_norm1[l]) * (1.0 + sc1) + sh1
        u = h @ w_in[l]
        q, k, v, ga, gb = jnp.split(
            u, [D_ATTN, 2 * D_ATTN, 3 * D_ATTN, 3 * D_ATTN + D_CONV], axis=-1)
        attn = dilated_attention(split_heads(q), split_heads(k), split_heads(v), slopes)
        attn = attn.transpose(0, 2, 1, 3).reshape(B, S, D_ATTN).astype(x.dtype)
        conv = conformer_conv(ga, gb, conv_w[l], conv_b[l], conv_ln_g[l], conv_ln_b[l])
        mix = jnp.concatenate([attn, conv], axis=-1) @ w_out[l]
        x = x + gt1 * mix

        h2 = rms_norm(x, g_norm2[l]) * (1.0 + sc2) + sh2
        x = x + gt2 * hier_moe(h2, w_router_group[l], b_router_group[l], w_router_expert[l],
                               b_router_expert[l], w_exp_gate[l], w_exp_up[l], w_exp_down[l])

    return rms_norm(x, g_final)
```

```python
import numpy as np
import ml_dtypes
from contextlib import ExitStack
import concourse.bass as bass
import concourse.mybir as mybir
from concourse.bass_utils import run_bass_kernel_spmd

dt = mybir.dt
AF = mybir.ActivationFunctionType
ALU = mybir.AluOpType
AX = mybir.AxisListType
F32 = dt.float32
BF16 = dt.bfloat16
I32 = dt.int32

S_LEN = 4096
D = 1024
NT = 32
DEPTH = 4
DIN = 2560
NH = 8
NE = 32
DE = 512
BS = 256
NB = 63
NSLOT = NB * BS
EPS = 1e-6
BIG = 1.0e30
DILS = (1, 4, 16)
POOLB = 206 * 1024

ENGS = ('sync', 'scalar', 'vector', 'gpsimd', 'tensor')
U8 = dt.uint8
_SZ = {F32: 4, BF16: 2, I32: 4, U8: 1}


class Op:
    __slots__ = ('eng', 'fn', 'deps', 'dma_key', 'ninst', 'has_dep', 'sigval', 'sem', 'idx')


class Sched:
    def __init__(self, nc):
        self.nc = nc
        self.ops = {e: [] for e in ENGS}
        self.last_w = {}
        self.readers = {}
        self.finals = []
        self.nops = 0
        self.since = []
        self.bar = None

    def op(self, eng, fn, reads=(), writes=(), dma_key=None, ninst=1, extra=()):
        o = Op()
        o.eng = eng
        o.fn = fn
        o.dma_key = dma_key
        o.ninst = ninst
        o.has_dep = False
        o.sigval = None
        o.sem = None
        o.idx = self.nops
        self.nops += 1
        ex = [r for r in reads if isinstance(r, str) and r.startswith('PS')]
        if ex:
            reads = [r for r in reads if r not in ex]
            writes = list(writes) + ex
        deps = {}
        raw = set()
        for r in reads:
            w = self.last_w.get(r)
            if w is not None:
                deps[id(w)] = w
                raw.add(id(w))
        for w_ in writes:
            w = self.last_w.get(w_)
            if w is not None:
                deps[id(w)] = w
            for rd in self.readers.get(w_, ()):
                deps[id(rd)] = rd
        for d in extra:
            deps[id(d)] = d
        if self.bar is not None:
            deps[id(self.bar)] = self.bar
        keep = []
        for d in deps.values():
            if d is o:
                continue
            if d.dma_key is None and d.eng == eng and dma_key is None:
                if eng == 'tensor' or id(d) not in raw:
                    continue
            keep.append(d)
            d.has_dep = True
        o.deps = keep
        for r in reads:
            self.readers.setdefault(r, []).append(o)
        for w_ in writes:
            self.last_w[w_] = o
            self.readers[w_] = []
        self.ops[eng].append(o)
        self.since.append(o)
        return o

    def barrier(self, fn, eng='vector'):
        last = {}
        for o in self.since:
            key = ('d', o.dma_key) if o.dma_key is not None else ('e', o.eng)
            last[key] = o
        prev = list(last.values())
        self.since = []
        self.last_w = {}
        self.readers = {}
        o = self.op(eng, fn, extra=prev)
        self.bar = o
        return o

    def final(self, o):
        o.has_dep = True
        self.finals.append(o)

    def emit(self, stack):
        nc = self.nc
        esem = {e: stack.enter_context(nc.semaphore("es_" + e)) for e in ENGS}
        dsem = {}
        dcount = {}
        for e in ENGS:
            cnt = 0
            for o in self.ops[e]:
                if o.dma_key is None and o.has_dep:
                    cnt += 1
                    o.sigval = cnt
                    o.sem = esem[e]
        dmaops = sorted([o for e in ENGS for o in self.ops[e] if o.dma_key is not None], key=lambda o: o.idx)
        for o in dmaops:
            k = o.dma_key
            if k not in dsem:
                dsem[k] = stack.enter_context(nc.semaphore("ds_" + str(k)))
                dcount[k] = 0
            dcount[k] += 16 * o.ninst
            o.sigval = dcount[k]
            o.sem = dsem[k]
        self.n_sems = len(esem) + len(dsem)
        block = stack.enter_context(nc.Block())
        finals = self.finals

        def run_engine(e, eng):
            waited = {}
            for o in self.ops[e]:
                need = {}
                for d in o.deps:
                    key = id(d.sem)
                    if key not in need or need[key][1] < d.sigval:
                        need[key] = (d.sem, d.sigval)
                for key, (s, v) in need.items():
                    if waited.get(key, 0) >= v:
                        continue
                    eng.wait_ge(s, v)
                    waited[key] = v
                r = o.fn(eng)
                if o.dma_key is not None:
                    ins = r if isinstance(r, (list, tuple)) else [r]
                    assert len(ins) == o.ninst, (len(ins), o.ninst)
                    for i_ in ins:
                        i_.then_inc(o.sem, 16)
                elif o.has_dep:
                    ins = r[-1] if isinstance(r, (list, tuple)) else r
                    ins.then_inc(o.sem, 1)
            if e == 'sync':
                need = {}
                for d in finals:
                    key = id(d.sem)
                    if key not in need or need[key][1] < d.sigval:
                        need[key] = (d.sem, d.sigval)
                for key, (s, v) in need.items():
                    eng.wait_ge(s, v)

        @block.sync
        def _(eng):
            run_engine('sync', eng)

        @block.scalar
        def _(eng):
            run_engine('scalar', eng)

        @block.vector
        def _(eng):
            run_engine('vector', eng)

        @block.gpsimd
        def _(eng):
            run_engine('gpsimd', eng)

        @block.tensor
        def _(eng):
            run_engine('tensor', eng)


class Mem:
    def __init__(self, pool, size):
        self.pool = pool
        self.size = size
        self.top = 0
        self.marks = []
        self.peak = 0

    def alloc(self, shape, dtype, p0=0, p1=128):
        n = 1
        for s in shape:
            n *= s
        nb = n * _SZ[dtype]
        off = (self.top + 31) // 32 * 32
        self.top = off + nb
        self.peak = max(self.peak, self.top)
        assert self.top <= self.size, ("SBUF pool overflow", self.top, self.size)
        ap = self.pool[p0:p1, off:off + nb]
        if dtype != U8:
            ap = ap.bitcast(dtype)
        if len(shape) == 2:
            ap = ap.rearrange("p (a b) -> p a b", a=shape[0])
        elif len(shape) == 3:
            ap = ap.rearrange("p (a b c) -> p a b c", a=shape[0], b=shape[1])
        elif len(shape) == 4:
            ap = ap.rearrange("p (a b c d) -> p a b c d", a=shape[0], b=shape[1], c=shape[2])
        return ap

    def mark(self):
        self.marks.append(self.top)

    def release(self):
        self.top = self.marks.pop()


class Ctx:
    pass


def _bnd(cx, e):
    if cx.regs.get('bnd') is None:
        cx.regs['bnd'] = e.to_reg(NSLOT - 1)
    return cx.regs['bnd']


def _bc(ap_row):
    b = ap_row.partition_broadcast(128)
    if len(b.shape) == 3:
        b = b.rearrange("p o n -> p (o n)")
    return b


def phase_prologue(S, cx):
    M = cx.mem
    cx.identF = M.alloc([128], F32)
    cx.identB = M.alloc([128], BF16)
    cx.onesB = M.alloc([128], BF16)
    cx.rstrictB = M.alloc([128], BF16)
    cx.onesD = M.alloc([128], F32)
    cx.scr = M.alloc([8], F32)
    cx.scr2 = M.alloc([8], F32)
    S.op('gpsimd', lambda e: e.memset(cx.scr2, 0.0), writes=['scr2'])
    S.op('sync', lambda e: e.dma_start(out=cx.identF, in_=cx.cf32[:, 0:128]), writes=['identF'], dma_key='c0')
    S.op('sync', lambda e: e.dma_start(out=cx.onesD, in_=cx.cf32[:, 128:256]), writes=['onesD'], dma_key='c1')
    S.op('sync', lambda e: e.dma_start(out=cx.identB, in_=cx.cbf[:, 0:128]), writes=['identB'], dma_key='c2')
    S.op('sync', lambda e: e.dma_start(out=cx.onesB, in_=cx.cbf[:, 128:256]), writes=['onesB'], dma_key='c3')
    S.op('sync', lambda e: e.dma_start(out=cx.rstrictB, in_=cx.cbf[:, 256:384]), writes=['rstrictB'], dma_key='c4')
    M.mark()
    cc = M.alloc([8], F32)
    cact = M.alloc([8], F32)
    cbc = M.alloc([8, 128], F32)
    bmod = M.alloc([6 * D], F32, 0, 1)
    modrow = M.alloc([6 * D], F32, 0, 1)
    stage = [M.alloc([8, 512], F32) for _ in range(2)]
    S.op('sync', lambda e: e.dma_start(out=cc, in_=cx.c_h.ap().rearrange("o (p k) -> (o p) k", k=8)), writes=['cc'], dma_key='c5')
    S.op('scalar', lambda e: e.activation(out=cact, in_=cc, func=AF.Silu), reads=['cc'], writes=['cact'])
    S.op('vector', lambda e: e.tensor_copy(out=cbc, in_=cact.unsqueeze(2).to_broadcast([128, 8, 128])), reads=['cact'], writes=['cbc'])
    it = 0
    for l in range(cx.n_layers):
        S.op('sync', lambda e, l=l: e.dma_start(out=bmod, in_=cx.b_mod[l:l + 1, :]), writes=['bmod'], dma_key='bm')
        for n in range(12):
            sl = it % 2
            it += 1
            src = cx.w_mod[l].rearrange("(p k) n -> p k n", k=8)[:, :, 512 * n:512 * n + 512]
            S.op('sync', lambda e, sl=sl, src=src: e.dma_start(out=stage[sl], in_=src), writes=['pst%d' % sl], dma_key='pst%d' % sl)
            ps = cx.psF[sl]
            for k in range(8):
                S.op('tensor', lambda e, k=k, sl=sl, ps=ps: e.matmul(ps[:, :], lhsT=cbc[:, k, :], rhs=stage[sl][:, k, :], start=(k == 0), stop=(k == 7)),
                     reads=['cbc', 'pst%d' % sl], writes=['PS%d' % sl])
            S.op('vector', lambda e, n=n, ps=ps: e.tensor_tensor(out=modrow[0:1, 512 * n:512 * n + 512], in0=ps[0:1, :], in1=bmod[0:1, 512 * n:512 * n + 512], op=ALU.add),
                 reads=['PS%d' % sl, 'bmod'], writes=['modrow'])
        S.op('sync', lambda e, l=l: e.dma_start(out=cx.modscr[l:l + 1, :], in_=modrow[0:1, :]), reads=['modrow'], writes=['modscr%d' % l], dma_key='ms')
    S.barrier(lambda e: e.memset(cx.scr, 0.0))
    M.release()


def load_mod_bc(S, cx, dst, l, idx, key):
    S.op('sync', lambda e: e.dma_start(out=dst, in_=_bc(cx.modscr[l:l + 1, idx * D:(idx + 1) * D])), writes=[key], dma_key='mb_' + key)


def norm_mod_tile(S, cx, xt, xkey, gp, sh, hb, hkey, j, ss, rs, junk, out_dt_note=''):
    def sq(e):
        i1 = e.activation(out=junk, in_=xt, func=AF.Square, accum_out=ss[:, j:j + 1])
        i2 = e.activation(out=cx.scr2[:, 0:1], in_=cx.scr2[:, 1:2], func=AF.Copy)
        return [i1, i2]
    S.op('scalar', sq, reads=[xkey], writes=['junk', 'ss%d' % j])
    S.op('scalar', lambda e: e.activation(out=rs[:, j:j + 1], in_=ss[:, j:j + 1], func=AF.Sqrt, bias=cx.epsT[:, 0:1], scale=1.0 / D), reads=['ss%d' % j], writes=['rs%d' % j])
    S.op('vector', lambda e: e.reciprocal(out=rs[:, j:j + 1], in_=rs[:, j:j + 1]), reads=['rs%d' % j], writes=['rs%d' % j])


def phase_m1(S, cx, l):
    M = cx.mem
    M.mark()
    g1 = M.alloc([D], F32)
    sc = M.alloc([D], F32)
    sh = M.alloc([D], F32)
    xt = [M.alloc([D], F32) for _ in range(2)]
    tmp = [M.alloc([D], F32) for _ in range(2)]
    hb = [M.alloc([D], BF16) for _ in range(2)]
    junk = M.alloc([D], BF16)
    ss = M.alloc([NT], F32)
    rs = M.alloc([NT], F32)
    hT = cx.hT
    S.op('sync', lambda e: e.dma_start(out=g1, in_=_bc(cx.g_norm1[l:l + 1, :])), writes=['g1'], dma_key='m1g')
    load_mod_bc(S, cx, sh, l, 0, 'sh1')
    load_mod_bc(S, cx, sc, l, 1, 'sc1')
    S.op('vector', lambda e: e.scalar_tensor_tensor(out=g1, in0=sc, scalar=1.0, in1=g1, op0=ALU.add, op1=ALU.mult), reads=['sc1', 'g1'], writes=['g1'])
    xsrc = cx.x_h if l == 0 else cx.xres

    def stage_a(j):
        b = j % 2
        S.op('sync', lambda e, j=j, b=b: e.dma_start(out=xt[b], in_=xsrc[128 * j:128 * j + 128, :]), reads=['xres'], writes=['xt%d' % b], dma_key='xt%d' % b)
        norm_mod_tile(S, cx, xt[b], 'xt%d' % b, g1, sh, None, None, j, ss, rs, junk)
        S.op('vector', lambda e, j=j, b=b: e.scalar_tensor_tensor(out=tmp[b], in0=xt[b], scalar=rs[:, j:j + 1], in1=g1, op0=ALU.mult, op1=ALU.mult),
             reads=['xt%d' % b, 'rs%d' % j, 'g1'], writes=['tmp%d' % b])
        S.op('gpsimd', lambda e, b=b: e.tensor_tensor(out=hb[b], in0=tmp[b], in1=sh, op=ALU.add), reads=['tmp%d' % b, 'sh1'], writes=['hb%d' % b])

    def stage_b(j):
        b = j % 2
        pT = cx.psB[b]
        for k in range(8):
            S.op('tensor', lambda e, k=k, b=b, pT=pT: e.transpose(pT[:, 128 * k:128 * k + 128], hb[b][:, k:D:8], cx.identB), reads=['hb%d' % b, 'identB'], writes=['PS%d' % b])
        S.op('scalar', lambda e, j=j, pT=pT: e.activation(out=hT[:, :, 128 * j:128 * j + 128], in_=pT.rearrange("p (k t) -> p k t", k=8), func=AF.Copy),
             reads=['PS%d' % b], writes=['hT'])
    stage_a(0)
    for j in range(NT):
        if j + 1 < NT:
            stage_a(j + 1)
        stage_b(j)
    S.barrier(lambda e: e.memset(cx.scr, 0.0))
    M.release()


def load_win_chunk(S, cx, l, oc, stg, skey, wbf, wkey):
    src = cx.w_in[l].rearrange("(p k) n -> p k n", k=8)[:, :, 128 * oc:128 * oc + 128]
    S.op('sync', lambda e: e.dma_start(out=stg, in_=src), writes=[skey], dma_key='ld_' + skey)
    S.op('gpsimd', lambda e: e.tensor_copy(out=wbf, in_=stg), reads=[skey], writes=[wkey])


def phase_m2(S, cx, l):
    M = cx.mem
    M.mark()
    hT = cx.hT
    uT = M.alloc([4, 30 + S_LEN], BF16)
    stg = [M.alloc([8, 128], F32) for _ in range(2)]
    wa = [M.alloc([8, 128], BF16) for _ in range(2)]
    wb = [M.alloc([8, 128], BF16) for _ in range(2)]
    sig = [M.alloc([512], F32) for _ in range(2)]
    cpk = M.alloc([512], F32, 0, 34)
    cw = M.alloc([4, 34], F32)
    for c in range(4):
        S.op('gpsimd', lambda e, c=c: e.memset(uT[:, c, 0:30], 0.0), writes=['uT%d' % c])
    S.op('sync', lambda e: e.dma_start(out=cpk[0:34, :], in_=cx.conv_pack[l]), writes=['cpk'], dma_key='cpk')
    pc = cx.psF[6]
    for c in range(4):
        S.op('tensor', lambda e, c=c: e.transpose(pc[:, 34 * c:34 * c + 34], cpk[0:34, 128 * c:128 * c + 128], cx.identF[0:34, 0:34]), reads=['cpk', 'identF'], writes=['PS6'])
    S.op('vector', lambda e: e.tensor_copy(out=cw, in_=pc[:, 0:136].rearrange("p (c r) -> p c r", c=4)), reads=['PS6'], writes=['cw'])
    it = 0
    for c in range(4):
        b = c % 2
        load_win_chunk(S, cx, l, 12 + c, stg[0], 'stgA', wa[b], 'wa%d' % b)
        load_win_chunk(S, cx, l, 16 + c, stg[1], 'stgB', wb[b], 'wb%d' % b)
        for tb in range(8):
            s2 = it % 2
            it += 1
            pa = cx.psF[s2]
            pb = cx.psF[2 + s2]
            for k in range(8):
                S.op('tensor', lambda e, k=k, b=b, tb=tb, pa=pa: e.matmul(pa[:, :], lhsT=wa[b][:, k, :], rhs=hT[:, k, 512 * tb:512 * tb + 512], start=(k == 0), stop=(k == 7)),
                     reads=['wa%d' % b, 'hT'], writes=['PS%d' % s2])
            for k in range(8):
                S.op('tensor', lambda e, k=k, b=b, tb=tb, pb=pb: e.matmul(pb[:, :], lhsT=wb[b][:, k, :], rhs=hT[:, k, 512 * tb:512 * tb + 512], start=(k == 0), stop=(k == 7)),
                     reads=['wb%d' % b, 'hT'], writes=['PS%d' % (2 + s2)])
            S.op('scalar', lambda e, s2=s2, pb=pb: e.activation(out=sig[s2], in_=pb[:, :], func=AF.Sigmoid), reads=['PS%d' % (2 + s2)], writes=['sig%d' % s2])
            S.op('vector', lambda e, c=c, tb=tb, s2=s2, pa=pa: e.tensor_tensor(out=uT[:, c, 30 + 512 * tb:30 + 512 * tb + 512], in0=pa[:, :], in1=sig[s2], op=ALU.mult),
                 reads=['PS%d' % s2, 'sig%d' % s2], writes=['uT%d' % c])
    dg = M.alloc([4, 31, 128], BF16)
    for c in range(4):
        for j in range(31):
            eng = 'vector' if (j % 2 == 0) else 'gpsimd'
            S.op(eng, lambda e, c=c, j=j: e.tensor_scalar(out=dg[:, c, j, :], in0=cx.identB, scalar1=cw[:, c, j:j + 1], scalar2=None, op0=ALU.mult),
                 reads=['identB', 'cw'], writes=['dg%d_%d' % (c, j)])
    ysb = M.alloc([4, 512], F32)
    y2 = M.alloc([4, 512], F32)
    mean = M.alloc([512], F32)
    var = M.alloc([512], F32)
    tz = [M.alloc([512], F32) for _ in range(2)]
    cv = [M.alloc([4, 512], BF16) for _ in range(2)]
    mixT_v = cx.mixT.rearrange("(k p) t -> p k t", p=128)
    for tb in range(8):
        for c in range(4):
            py = cx.psF[c]
            for j in range(31):
                S.op('tensor', lambda e, c=c, j=j, tb=tb, py=py: e.matmul(py[:, :], lhsT=dg[:, c, j, :], rhs=uT[:, c, 512 * tb + j:512 * tb + j + 512], start=(j == 0), stop=(j == 30)),
                     reads=['dg%d_%d' % (c, j), 'uT%d' % c], writes=['PS%d' % c])
            S.op('scalar', lambda e, c=c, py=py: e.activation(out=ysb[:, c, :], in_=py[:, :], func=AF.Identity, bias=cw[:, c, 31:32]), reads=['PS%d' % c, 'cw'], writes=['ysb%d' % c])
            S.op('scalar', lambda e, c=c, py=py: e.activation(out=y2[:, c, :], in_=py[:, :], func=AF.Square, bias=cw[:, c, 31:32]), reads=['PS%d' % c, 'cw'], writes=['y2%d' % c])
        pm = cx.psF[4]
        pq = cx.psF[5]
        for c in range(4):
            S.op('tensor', lambda e, c=c: e.matmul(pm[:, :], lhsT=cx.onesD, rhs=ysb[:, c, :], start=(c == 0), stop=(c == 3)), reads=['onesD', 'ysb%d' % c], writes=['PS4'])
        for c in range(4):
            S.op('tensor', lambda e, c=c: e.matmul(pq[:, :], lhsT=cx.onesD, rhs=y2[:, c, :], start=(c == 0), stop=(c == 3)), reads=['onesD', 'y2%d' % c], writes=['PS5'])
        S.op('scalar', lambda e: e.activation(out=mean, in_=pm[:, :], func=AF.Copy), reads=['PS4'], writes=['mean'])
        S.op('vector', lambda e: e.tensor_tensor(out=var, in0=pm[:, :], in1=mean, op=ALU.mult), reads=['PS4', 'mean'], writes=['var'])
        S.op('vector', lambda e: e.tensor_tensor(out=var, in0=pq[:, :], in1=var, op=ALU.subtract), reads=['PS5', 'var'], writes=['var'])
        S.op('scalar', lambda e: e.activation(out=var, in_=var, func=AF.Sqrt, bias=cx.epsT[:, 0:1], scale=1.0), reads=['var'], writes=['var'])
        S.op('vector', lambda e: e.reciprocal(out=var, in_=var), reads=['var'], writes=['var'])
        cb = tb % 2
        for c in range(4):
            z = tz[c % 2]
            zk = 'tz%d' % (c % 2)
            S.op('vector', lambda e, c=c, z=z: e.tensor_tensor(out=z, in0=ysb[:, c, :], in1=mean, op=ALU.subtract), reads=['ysb%d' % c, 'mean'], writes=[zk])
            S.op('vector', lambda e, z=z: e.tensor_tensor(out=z, in0=z, in1=var, op=ALU.mult), reads=[zk, 'var'], writes=[zk])
            S.op('scalar', lambda e, c=c, z=z, cb=cb: e.activation(out=cv[cb][:, c, :], in_=z, func=AF.Silu, bias=cw[:, c, 33:34], scale=cw[:, c, 32:33]), reads=[zk, 'cw'], writes=['cv%d' % cb])
        S.op('gpsimd', lambda e, tb=tb, cb=cb: e.dma_start(out=mixT_v[:, 4:8, 512 * tb:512 * tb + 512], in_=cv[cb]), reads=['cv%d' % cb], writes=['mixT'], dma_key='cvst%d' % cb)
    S.barrier(lambda e: e.memset(cx.scr, 0.0))
    M.release()


def qblocks(dil, half):
    nbr = 32 // dil
    hb = nbr // 2
    return [(r, n) for r in range(dil) for n in range(half * hb, half * hb + hb)]


def phase_m3(S, cx, l):
    M = cx.mem
    M.mark()
    hT = cx.hT
    wtab = M.alloc([NH, 3, 256], BF16)
    QT = M.alloc([S_LEN], BF16)
    KT = M.alloc([S_LEN], BF16)
    VT = M.alloc([S_LEN], BF16)
    Vt = M.alloc([3 * 32 * 256], BF16)
    acc = [M.alloc([2048], F32) for _ in range(2)]
    Et = [M.alloc([256], F32) for _ in range(4)]
    Pt = [M.alloc([256], BF16) for _ in range(4)]
    rd = M.alloc([2048], F32)
    aT = [M.alloc([2048], BF16) for _ in range(2)]
    stg = [M.alloc([8, 128], F32) for _ in range(2)]
    wq = [M.alloc([8, 128], BF16) for _ in range(3)]
    S.op('sync', lambda e: e.dma_start(out=wtab, in_=cx.wtab_h.ap().rearrange("p (h b c) -> p h b c", h=NH, b=3)), writes=['wtab'], dma_key='wtab')
    S.op('gpsimd', lambda e: e.memset(Vt, 1.0), writes=['Vt'])
    mixT_v = cx.mixT.rearrange("(k p) t -> p k t", p=128)
    pcount = 0
    ecount = 0
    ocount = 0
    for hp in range(4):
        for wi, (oc, dst, name) in enumerate(((hp, QT, 'QT'), (4 + hp, KT, 'KT'), (8 + hp, VT, 'VT'))):
            load_win_chunk(S, cx, l, oc, stg[wi % 2], 'stg%d' % (wi % 2), wq[wi], 'wq%d' % wi)
            for tb in range(8):
                s2 = pcount % 2
                pcount += 1
                pp = cx.psF[s2]
                for k in range(8):
                    S.op('tensor', lambda e, k=k, wi=wi, tb=tb, pp=pp: e.matmul(pp[:, :], lhsT=wq[wi][:, k, :], rhs=hT[:, k, 512 * tb:512 * tb + 512], start=(k == 0), stop=(k == 7)),
                         reads=['wq%d' % wi, 'hT'], writes=['PS%d' % s2])
                if name == 'QT':
                    S.op('scalar', lambda e, tb=tb, pp=pp, dst=dst: e.activation(out=dst[:, 512 * tb:512 * tb + 512], in_=pp[:, :], func=AF.Copy, scale=0.125),
                         reads=['PS%d' % s2], writes=[name])
                else:
                    S.op('vector', lambda e, tb=tb, pp=pp, dst=dst: e.tensor_copy(out=dst[:, 512 * tb:512 * tb + 512], in_=pp[:, :]), reads=['PS%d' % s2], writes=[name])
        for di, dil in enumerate(DILS):
            nbr = 32 // dil
            for g in range(8):
                pv = cx.psB[g % 2]
                for q4 in range(4):
                    blk = 4 * g + q4
                    r, n = blk // nbr, blk % nbr
                    t0 = r + dil * 128 * n
                    S.op('tensor', lambda e, q4=q4, t0=t0, dil=dil, pv=pv: e.transpose(pv[:, 128 * q4:128 * q4 + 128], VT[:, t0:t0 + dil * 127 + 1:dil], cx.identB),
                         reads=['VT', 'identB'], writes=['PS%d' % (g % 2)])
                vdst = bass.AP(Vt.tensor, Vt.offset + di * 8192 + g * 1024, [list(Vt.ap[0]), [256, 4], [192, 2], [1, 64]])
                vsrc = pv[:, 0:512].rearrange("p (q s d) -> p q s d", q=4, s=2)
                S.op('scalar' if g % 2 == 0 else 'vector',
                     (lambda e, vdst=vdst, vsrc=vsrc: e.activation(out=vdst, in_=vsrc, func=AF.Copy)) if g % 2 == 0 else
                     (lambda e, vdst=vdst, vsrc=vsrc: e.tensor_copy(out=vdst, in_=vsrc)),
                     reads=['PS%d' % (g % 2)], writes=['Vt'])
        for s in range(2):
            h = 2 * hp + s
            p0, p1 = 64 * s, 64 * s + 64
            for half in range(2):
                ab = (2 * s + half) % 2
                A = acc[ab]
                akey = 'acc%d' % ab
                blist = [(di, dil, r, n) for di, dil in enumerate(DILS) for (r, n) in qblocks(dil, half)]
                LAG = 3
                pend = {}
                for t in range(len(blist) + LAG):
                    if t < len(blist):
                        di, dil, r, n = blist[t]
                        nbr = 32 // dil
                        has_prev = n > 0
                        lo = 0 if has_prev else 128
                        tq = r + dil * 128 * n
                        qs = QT[p0:p1, tq:tq + dil * 127 + 1:dil]
                        e3 = ecount % 4
                        ecount += 1
                        pS = cx.psF[2 + e3][:, 0:256]
                        pskey = 'PS%d' % (2 + e3)
                        if has_prev:
                            tk = r + dil * 128 * (n - 1)
                            S.op('tensor', lambda e, tk=tk, qs=qs, pS=pS, dil=dil, p0=p0, p1=p1: e.matmul(pS[:, 0:128], lhsT=KT[p0:p1, tk:tk + dil * 127 + 1:dil], rhs=qs, start=True, stop=True),
                                 reads=['KT', 'QT'], writes=[pskey])
                        S.op('tensor', lambda e, tq=tq, qs=qs, pS=pS, dil=dil, p0=p0, p1=p1: e.matmul(pS[:, 128:256], lhsT=KT[p0:p1, tq:tq + dil * 127 + 1:dil], rhs=qs, start=True, stop=True),
                             reads=['KT', 'QT'], writes=[pskey])
                        S.op('scalar', lambda e, e3=e3, lo=lo, pS=pS: e.activation(out=Et[e3][:, lo:256], in_=pS[:, lo:256], func=AF.Exp), reads=[pskey], writes=['Et%d' % e3])
                        S.op('vector' if (ecount % 2 == 0) else 'gpsimd', lambda e, e3=e3, lo=lo, h=h, di=di: e.tensor_tensor(out=Pt[e3][:, lo:256], in0=Et[e3][:, lo:256], in1=wtab[:, h, di, lo:256], op=ALU.mult),
                             reads=['Et%d' % e3, 'wtab'], writes=['Pt%d' % e3])
                        pend[t] = (di, dil, r, n, e3, has_prev, tq)
                    if t - LAG >= 0:
                        di, dil, r, n, e3, has_prev, tq = pend.pop(t - LAG)
                        nbr = 32 // dil
                        o4 = ocount % 2
                        ocount += 1
                        pO = cx.psF[6 + o4][:, 0:128]
                        pokey = 'PS%d' % (6 + o4)
                        blk = r * nbr + n

                        def vaug(b_, di=di):
                            voff = di * 8192 + 256 * b_ + 128 * s
                            return Vt[:, voff:voff + 128]
                        if has_prev:
                            va = vaug(blk - 1)
                            S.op('tensor', lambda e, va=va, e3=e3, pO=pO: e.matmul(pO, lhsT=va, rhs=Pt[e3][:, 0:128], start=True, stop=False), reads=['Vt', 'Pt%d' % e3], writes=[pokey])
                        vb = vaug(blk)
                        S.op('tensor', lambda e, vb=vb, e3=e3, pO=pO, has_prev=has_prev: e.matmul(pO, lhsT=vb, rhs=Pt[e3][:, 128:256], start=(not has_prev), stop=True),
                             reads=['Vt', 'Pt%d' % e3], writes=[pokey])
                        tl = tq - 2048 * half
                        dstA = A[:, tl:tl + dil * 127 + 1:dil]
                        if di == 0:
                            S.op('scalar', lambda e, dstA=dstA, pO=pO: e.activation(out=dstA, in_=pO, func=AF.Copy), reads=[pokey], writes=[akey])
                        else:
                            S.op('vector', lambda e, dstA=dstA, pO=pO: e.tensor_tensor(out=dstA, in0=pO, in1=dstA, op=ALU.add), reads=[pokey, akey], writes=[akey])
                if s == 0:
                    nlo, dlo = 0, 64
                else:
                    nlo, dlo = 64, 0
                S.op('vector', lambda e, A=A, dlo=dlo: e.reciprocal(out=A[dlo:dlo + 64, :], in_=A[dlo:dlo + 64, :]), reads=[akey], writes=[akey])
                S.op('scalar', lambda e, A=A, dlo=dlo, nlo=nlo: e.activation(out=rd[nlo:nlo + 64, :], in_=A[dlo:dlo + 64, :], func=AF.Copy), reads=[akey], writes=['rd'])
                S.op('vector', lambda e, A=A, nlo=nlo, half=half: e.tensor_tensor(out=aT[half][nlo:nlo + 64, :], in0=A[nlo:nlo + 64, :], in1=rd[nlo:nlo + 64, :], op=ALU.mult),
                     reads=[akey, 'rd'], writes=['aT%d_%d' % (half, s)])
        for half in range(2):
            S.op('gpsimd', lambda e, hp=hp, half=half: e.dma_start(out=mixT_v[:, hp, 2048 * half:2048 * half + 2048], in_=aT[half]),
                 reads=['aT%d_0' % half, 'aT%d_1' % half], writes=['mixT'], dma_key='atst%d' % half)
    S.barrier(lambda e: e.memset(cx.scr, 0.0))
    M.release()


def phase_m4(S, cx, l):
    M = cx.mem
    M.mark()
    gt = M.alloc([D], F32)
    wo = M.alloc([8, D], BF16)
    stg = [M.alloc([2, D], F32) for _ in range(2)]
    mT = [M.alloc([8, 512], BF16) for _ in range(2)]
    xt = [M.alloc([D], F32) for _ in range(2)]
    xn = [M.alloc([D], F32) for _ in range(2)]
    load_mod_bc(S, cx, gt, l, 2, 'gt1')
    wsrc = cx.w_out[l].rearrange("(k p) n -> p k n", p=128)
    for q in range(4):
        S.op('sync', lambda e, q=q: e.dma_start(out=stg[q % 2], in_=wsrc[:, 2 * q:2 * q + 2, :]), writes=['wstg%d' % (q % 2)], dma_key='wstg%d' % (q % 2))
        S.op('gpsimd', lambda e, q=q: e.tensor_copy(out=wo[:, 2 * q:2 * q + 2, :], in_=stg[q % 2]), reads=['wstg%d' % (q % 2)], writes=['wo'])
    mixT_v = cx.mixT.rearrange("(k p) t -> p k t", p=128)
    xsrc = cx.x_h if l == 0 else cx.xres
    for tb in range(8):
        mb = tb % 2
        S.op('sync', lambda e, tb=tb, mb=mb: e.dma_start(out=mT[mb], in_=mixT_v[:, :, 512 * tb:512 * tb + 512]), reads=['mixT'], writes=['mT%d' % mb], dma_key='mT%d' % mb)
        for jj in range(4):
            j = 4 * tb + jj
            b = j % 2
            S.op('sync', lambda e, j=j, b=b: e.dma_start(out=xt[b], in_=xsrc[128 * j:128 * j + 128, :]), reads=['xres%d' % j], writes=['xt%d' % b], dma_key='xt%d' % b)
            for nh in range(2):
                po = cx.psF[2 * b + nh]
                for kc in range(8):
                    S.op('tensor', lambda e, kc=kc, jj=jj, nh=nh, mb=mb, po=po: e.matmul(po[:, :], lhsT=mT[mb][:, kc, 128 * jj:128 * jj + 128], rhs=wo[:, kc, 512 * nh:512 * nh + 512], start=(kc == 0), stop=(kc == 7)),
                         reads=['mT%d' % mb, 'wo'], writes=['PS%d' % (2 * b + nh)])
                S.op('vector', lambda e, nh=nh, b=b, po=po: e.tensor_tensor(out=xn[b][:, 512 * nh:512 * nh + 512], in0=po[:, :], in1=gt[:, 512 * nh:512 * nh + 512], op=ALU.mult),
                     reads=['PS%d' % (2 * b + nh), 'gt1'], writes=['xn%d' % b])
            S.op('gpsimd', lambda e, b=b: e.tensor_tensor(out=xn[b], in0=xn[b], in1=xt[b], op=ALU.add), reads=['xn%d' % b, 'xt%d' % b], writes=['xn%d' % b])
            S.op('scalar', lambda e, j=j, b=b: e.dma_start(out=cx.xres[128 * j:128 * j + 128, :], in_=xn[b]), reads=['xn%d' % b], writes=['xres%d' % j], dma_key='xst%d' % b)
    S.barrier(lambda e: e.memset(cx.scr, 0.0))
    M.release()


def phase_moe(S, cx, l, last):
    M = cx.mem
    st = cx.stack
    M.mark()
    desti = M.alloc([NT, 2], I32)
    wts = M.alloc([NT, 2], F32)
    ebi = M.alloc([64], I32)
    offi = M.alloc([64, 4], I32)
    M.mark()
    h2b = M.alloc([NT, D], BF16)
    g2 = M.alloc([D], F32)
    sc = M.alloc([D], F32)
    sh = M.alloc([D], F32)
    xt = [M.alloc([D], F32) for _ in range(2)]
    tmp = [M.alloc([D], F32) for _ in range(2)]
    h2f = [M.alloc([D], F32) for _ in range(2)]
    h2T = [M.alloc([8, 128], F32) for _ in range(2)]
    junk = M.alloc([D], BF16)
    ss = M.alloc([NT], F32)
    rs = M.alloc([NT], F32)
    wr = M.alloc([8, 36], F32)
    brb = M.alloc([36], F32)
    LG = M.alloc([NT, 36], F32)
    S.op('sync', lambda e: e.dma_start(out=g2, in_=_bc(cx.g_norm2[l:l + 1, :])), writes=['g2'], dma_key='e1g')
    load_mod_bc(S, cx, sh, l, 3, 'sh2')
    load_mod_bc(S, cx, sc, l, 4, 'sc2')
    S.op('vector', lambda e: e.scalar_tensor_tensor(out=g2, in0=sc, scalar=1.0, in1=g2, op0=ALU.add, op1=ALU.mult), reads=['sc2', 'g2'], writes=['g2'])
    S.op('sync', lambda e: e.dma_start(out=wr, in_=cx.w_r[l].rearrange("(p k) n -> p k n", k=8)), writes=['wr'], dma_key='e1w')
    S.op('sync', lambda e: e.dma_start(out=brb, in_=_bc(cx.b_r[l:l + 1, :])), writes=['brb'], dma_key='e1b')
    def e1_a(j):
        b = j % 2
        S.op('sync', lambda e, j=j, b=b: e.dma_start(out=xt[b], in_=cx.moe_x[128 * j:128 * j + 128, :]), reads=['xres%d' % j], writes=['xt%d' % b], dma_key='xt%d' % b)
        norm_mod_tile(S, cx, xt[b], 'xt%d' % b, g2, sh, None, None, j, ss, rs, junk)
        S.op('vector', lambda e, j=j, b=b: e.scalar_tensor_tensor(out=tmp[b], in0=xt[b], scalar=rs[:, j:j + 1], in1=g2, op0=ALU.mult, op1=ALU.mult),
             reads=['xt%d' % b, 'rs%d' % j, 'g2'], writes=['tmp%d' % b])
        S.op('gpsimd', lambda e, b=b: e.tensor_tensor(out=h2f[b], in0=tmp[b], in1=sh, op=ALU.add), reads=['tmp%d' % b, 'sh2'], writes=['h2f%d' % b])
        S.op('gpsimd', lambda e, j=j, b=b: e.tensor_copy(out=h2b[:, j, :], in_=h2f[b]), reads=['h2f%d' % b], writes=['h2b%d' % j])

    def e1_b(j):
        b = j % 2
        for half in range(2):
            pT = cx.psF[2 * b + half]
            for k4 in range(4):
                k = 4 * half + k4
                S.op('tensor', lambda e, k=k, k4=k4, b=b, pT=pT: e.transpose(pT[:, 128 * k4:128 * k4 + 128], h2f[b][:, k:D:8], cx.identF), reads=['h2f%d' % b, 'identF'], writes=['PS%d' % (2 * b + half)])
            S.op('scalar' if half == 0 else 'vector',
                 (lambda e, b=b, half=half, pT=pT: e.activation(out=h2T[b][:, 4 * half:4 * half + 4, :], in_=pT.rearrange("p (k t) -> p k t", k=4), func=AF.Copy)) if half == 0 else
                 (lambda e, b=b, half=half, pT=pT: e.tensor_copy(out=h2T[b][:, 4 * half:4 * half + 4, :], in_=pT.rearrange("p (k t) -> p k t", k=4))),
                 reads=['PS%d' % (2 * b + half)], writes=['h2T%d_%d' % (b, half)])
        pl = cx.psF[4 + b]
        for k in range(8):
            S.op('tensor', lambda e, k=k, b=b, pl=pl: e.matmul(pl[:, 0:36], lhsT=h2T[b][:, k, :], rhs=wr[:, k, :], start=(k == 0), stop=(k == 7)),
                 reads=['h2T%d_%d' % (b, k // 4), 'wr'], writes=['PS%d' % (4 + b)])
        S.op('vector', lambda e, j=j, pl=pl: e.tensor_tensor(out=LG[:, j, :], in0=pl[:, 0:36], in1=brb, op=ALU.add), reads=['PS%d' % (4 + b), 'brb'], writes=['LG'])
    e1_a(0)
    for j in range(NT):
        if j + 1 < NT:
            e1_a(j + 1)
        e1_b(j)
    if cx.dbg:
        S.op('sync', lambda e: e.dma_start(out=cx.dbg_h2.ap().rearrange("(j p) d -> p j d", p=128), in_=h2b), reads=['h2b%d' % j for j in range(NT)], writes=['dbg_h2'], dma_key='dbg5')
        S.op('sync', lambda e: e.dma_start(out=cx.dbg_rs.ap(), in_=rs), reads=['rs%d' % j for j in range(NT)], writes=['dbg_rs'], dma_key='dbg6')
    gmx = M.alloc([NT], F32)
    gsel = M.alloc([NT, 4], F32)
    gex = M.alloc([NT, 4], F32)
    pg = M.alloc([NT], F32)
    EM = M.alloc([NT, 32], F32)
    m1 = M.alloc([NT], F32)
    m2 = M.alloc([NT], F32)
    sel1 = M.alloc([NT, 32], F32)
    sel2 = M.alloc([NT, 32], F32)
    Ab = M.alloc([NT, 32], BF16)
    Rk = M.alloc([NT, 32], F32)
    Tot = M.alloc([NT, 32], F32)
    TP = M.alloc([NT, 32], F32)
    cnt = M.alloc([32], F32)
    TH = M.alloc([32, 16], F32)
    BI = M.alloc([NB, 32], F32)
    cmp1 = M.alloc([32, 16], F32)
    cmp2 = M.alloc([NB, 32], F32)
    nblk = M.alloc([32], F32)
    bend = M.alloc([32], F32)
    base = M.alloc([32], F32)
    one32 = M.alloc([32], F32)
    ebf = M.alloc([64], F32)
    destf = M.alloc([NT, 2], F32)
    t1 = M.alloc([NT], F32)
    S.op('sync', lambda e: e.dma_start(out=TH, in_=cx.cf32[:, 256:256 + 512].rearrange("p (a b) -> p a b", a=32)), writes=['TH'], dma_key='e2a')
    S.op('sync', lambda e: e.dma_start(out=BI, in_=cx.cf32[:, 768:768 + NB * 32].rearrange("p (a b) -> p a b", a=NB)), writes=['BI'], dma_key='e2b')
    V = 'vector'
    G = LG[:, :, 0:4]
    EL = LG[:, :, 4:36]
    S.op(V, lambda e: e.tensor_reduce(out=gmx, in_=G, axis=AX.X, op=ALU.max), reads=['LG'], writes=['gmx'])
    S.op(V, lambda e: e.tensor_tensor(out=gsel, in0=G, in1=gmx.unsqueeze(2).to_broadcast([128, NT, 4]), op=ALU.is_equal), reads=['LG', 'gmx'], writes=['gsel'])
    S.op(V, lambda e: e.tensor_tensor(out=gex, in0=G, in1=gmx.unsqueeze(2).to_broadcast([128, NT, 4]), op=ALU.subtract), reads=['LG', 'gmx'], writes=['gex'])
    S.op('scalar', lambda e: e.activation(out=gex, in_=gex, func=AF.Exp), reads=['gex'], writes=['gex'])
    S.op(V, lambda e: e.tensor_reduce(out=pg, in_=gex, axis=AX.X, op=ALU.add), reads=['gex'], writes=['pg'])
    S.op(V, lambda e: e.reciprocal(out=pg, in_=pg), reads=['pg'], writes=['pg'])
    S.op(V, lambda e: e.tensor_scalar(out=gex, in0=gsel, scalar1=BIG, scalar2=-BIG, op0=ALU.mult, op1=ALU.add), reads=['gsel', 'gex'], writes=['gex'])
    S.op(V, lambda e: e.tensor_tensor(out=EM.rearrange("p j (g x) -> p j g x", g=4), in0=EL.rearrange("p j (g x) -> p j g x", g=4),
                                      in1=gex.unsqueeze(3).to_broadcast([128, NT, 4, 8]), op=ALU.add), reads=['LG', 'gex'], writes=['EM'])
    S.op(V, lambda e: e.tensor_reduce(out=m1, in_=EM, axis=AX.X, op=ALU.max), reads=['EM'], writes=['m1'])
    S.op(V, lambda e: e.tensor_tensor(out=sel1, in0=EM, in1=m1.unsqueeze(2).to_broadcast([128, NT, 32]), op=ALU.is_equal), reads=['EM', 'm1'], writes=['sel1'])
    S.op(V, lambda e: e.scalar_tensor_tensor(out=EM, in0=sel1, scalar=-BIG, in1=EM, op0=ALU.mult, op1=ALU.add), reads=['sel1', 'EM'], writes=['EM'])
    S.op(V, lambda e: e.tensor_reduce(out=m2, in_=EM, axis=AX.X, op=ALU.max), reads=['EM'], writes=['m2'])
    S.op(V, lambda e: e.tensor_tensor(out=sel2, in0=EM, in1=m2.unsqueeze(2).to_broadcast([128, NT, 32]), op=ALU.is_equal), reads=['EM', 'm2'], writes=['sel2'])
    S.op(V, lambda e: e.tensor_tensor(out=t1, in0=m2, in1=m1, op=ALU.subtract), reads=['m1', 'm2'], writes=['t1'])
    S.op('scalar', lambda e: e.activation(out=t1, in_=t1, func=AF.Exp), reads=['t1'], writes=['t1'])
    S.op(V, lambda e: e.tensor_scalar(out=t1, in0=t1, scalar1=1.0, scalar2=None, op0=ALU.add), reads=['t1'], writes=['t1'])
    S.op(V, lambda e: e.reciprocal(out=t1, in_=t1), reads=['t1'], writes=['t1'])
    S.op(V, lambda e: e.tensor_tensor(out=wts[:, :, 0], in0=t1, in1=pg, op=ALU.mult), reads=['t1', 'pg'], writes=['wts'])
    S.op(V, lambda e: e.tensor_tensor(out=wts[:, :, 1], in0=pg, in1=wts[:, :, 0], op=ALU.subtract), reads=['pg', 'wts'], writes=['wts'])
    S.op(V, lambda e: e.tensor_tensor(out=Ab, in0=sel1, in1=sel2, op=ALU.add), reads=['sel1', 'sel2'], writes=['Ab'])
    Abf = Ab.rearrange("p j x -> p (j x)")
    for hh in range(2):
        pr = cx.psF[hh]
        pt = cx.psF[2 + hh]
        S.op('tensor', lambda e, hh=hh, pr=pr: e.matmul(pr[:, :], lhsT=cx.rstrictB, rhs=Abf[:, 512 * hh:512 * hh + 512], start=True, stop=True), reads=['rstrictB', 'Ab'], writes=['PS%d' % hh])
        S.op('tensor', lambda e, hh=hh, pt=pt: e.matmul(pt[:, :], lhsT=cx.onesB, rhs=Abf[:, 512 * hh:512 * hh + 512], start=True, stop=True), reads=['onesB', 'Ab'], writes=['PS%d' % (2 + hh)])
        S.op('scalar', lambda e, hh=hh, pr=pr: e.activation(out=Rk.rearrange("p j x -> p (j x)")[:, 512 * hh:512 * hh + 512], in_=pr[:, :], func=AF.Copy), reads=['PS%d' % hh], writes=['Rk'])
        S.op(V, lambda e, hh=hh, pt=pt: e.tensor_copy(out=Tot.rearrange("p j x -> p (j x)")[:, 512 * hh:512 * hh + 512], in_=pt[:, :]), reads=['PS%d' % (2 + hh)], writes=['Tot'])
    S.op(V, lambda e: e.memset(TP[:, 0, :], 0.0), writes=['TP'])
    for j in range(1, NT):
        S.op(V, lambda e, j=j: e.tensor_tensor(out=TP[:, j, :], in0=TP[:, j - 1, :], in1=Tot[:, j - 1, :], op=ALU.add), reads=['TP', 'Tot'], writes=['TP'])
    S.op(V, lambda e: e.tensor_tensor(out=cnt, in0=TP[:, NT - 1, :], in1=Tot[:, NT - 1, :], op=ALU.add), reads=['TP', 'Tot'], writes=['cnt'])
    S.op(V, lambda e: e.tensor_tensor(out=cmp1, in0=cnt.unsqueeze(2).to_broadcast([128, 32, 16]), in1=TH, op=ALU.is_gt), reads=['cnt', 'TH'], writes=['cmp1'])
    S.op(V, lambda e: e.tensor_reduce(out=nblk, in_=cmp1, axis=AX.X, op=ALU.add), reads=['cmp1'], writes=['nblk'])
    S.op(V, lambda e: e.memset(one32, 1.0), writes=['one32'])
    S.op(V, lambda e: e.tensor_tensor_scan(out=bend, data0=one32, data1=nblk, initial=0.0, op0=ALU.mult, op1=ALU.add), reads=['one32', 'nblk'], writes=['bend'])
    S.op(V, lambda e: e.tensor_tensor(out=base, in0=bend, in1=nblk, op=ALU.subtract), reads=['bend', 'nblk'], writes=['base'])
    S.op(V, lambda e: e.tensor_scalar(out=base, in0=base, scalar1=float(BS), scalar2=None, op0=ALU.mult), reads=['base'], writes=['base'])
    S.op(V, lambda e: e.tensor_tensor(out=Rk, in0=Rk, in1=TP, op=ALU.add), reads=['Rk', 'TP'], writes=['Rk'])
    S.op(V, lambda e: e.tensor_tensor(out=Rk, in0=Rk, in1=base.unsqueeze(1).to_broadcast([128, NT, 32]), op=ALU.add), reads=['Rk', 'base'], writes=['Rk'])
    S.op(V, lambda e: e.tensor_tensor(out=sel1, in0=sel1, in1=Rk, op=ALU.mult), reads=['sel1', 'Rk'], writes=['sel1'])
    S.op(V, lambda e: e.tensor_tensor(out=sel2, in0=sel2, in1=Rk, op=ALU.mult), reads=['sel2', 'Rk'], writes=['sel2'])
    S.op(V, lambda e: e.tensor_reduce(out=destf[:, :, 0], in_=sel1, axis=AX.X, op=ALU.add), reads=['sel1'], writes=['destf'])
    S.op(V, lambda e: e.tensor_reduce(out=destf[:, :, 1], in_=sel2, axis=AX.X, op=ALU.add), reads=['sel2'], writes=['destf'])
    S.op(V, lambda e: e.tensor_copy(out=desti, in_=destf), reads=['destf'], writes=['desti'])
    S.op(V, lambda e: e.tensor_tensor(out=cmp2, in0=bend.unsqueeze(1).to_broadcast([128, NB, 32]), in1=BI, op=ALU.is_le), reads=['bend', 'BI'], writes=['cmp2'])
    S.op(V, lambda e: e.memset(ebf, 31.0), writes=['ebf'])
    S.op(V, lambda e: e.tensor_reduce(out=ebf[:, 0:NB], in_=cmp2, axis=AX.X, op=ALU.add), reads=['cmp2'], writes=['ebf'])
    einv = M.alloc([64], F32)
    S.op(V, lambda e: e.tensor_scalar(out=einv, in0=ebf, scalar1=31.5, scalar2=268435456.0, op0=ALU.is_gt, op1=ALU.mult), reads=['ebf'], writes=['einv'])
    S.op(V, lambda e: e.tensor_scalar(out=ebf, in0=ebf, scalar1=31.0, scalar2=None, op0=ALU.min), reads=['ebf'], writes=['ebf'])
    S.op(V, lambda e: e.tensor_copy(out=ebi, in_=ebf), reads=['ebf'], writes=['ebi'])
    offf = M.alloc([64, 4], F32)
    for q in range(4):
        S.op(V, lambda e, q=q: e.tensor_scalar(out=offf[:, :, q], in0=ebf, scalar1=2097152.0, scalar2=float(l * 32 * 2097152 + ((q % 2) * 8192 if q < 2 else (q % 2) * 1048576)), op0=ALU.mult, op1=ALU.add), reads=['ebf'], writes=['offf'])
    S.op(V, lambda e: e.tensor_tensor(out=offf, in0=offf, in1=einv.unsqueeze(2).to_broadcast([128, 64, 4]), op=ALU.add), reads=['offf', 'einv'], writes=['offf'])
    S.op(V, lambda e: e.tensor_copy(out=offi, in_=offf), reads=['offf'], writes=['offi'])
    if cx.dbg:
        S.op('sync', lambda e: e.dma_start(out=cx.dbg_lg.ap().rearrange("(j p) n -> p j n", p=128), in_=LG), reads=['LG'], writes=['dbg_lg'], dma_key='dbg1')
        S.op('sync', lambda e: e.dma_start(out=cx.dbg_dest.ap().rearrange("(j p) n -> p j n", p=128), in_=desti), reads=['desti'], writes=['dbg_dest'], dma_key='dbg2')
        S.op('sync', lambda e: e.dma_start(out=cx.dbg_wts.ap().rearrange("(j p) n -> p j n", p=128), in_=wts), reads=['wts'], writes=['dbg_wts'], dma_key='dbg3')
        S.op('sync', lambda e: e.dma_start(out=cx.dbg_eb.ap(), in_=ebi[0:1, :]), reads=['ebi'], writes=['dbg_eb'], dma_key='dbg4')
    if getattr(cx, 'moe_stop', None) == 'e2':
        S.barrier(lambda e: e.memset(cx.scr, 0.0))
        M.release()
        M.release()
        return
    for j in range(NT):
        for k in range(2):
            S.op('gpsimd', lambda e, j=j, k=k: e.indirect_dma_start(out=cx.xslots[:, :], out_offset=bass.IndirectOffsetOnAxis(ap=desti[:, j, k:k + 1], axis=0),
                                                                    in_=h2b[:, j, :], in_offset=None, bounds_check=_bnd(cx, e), oob_is_err=False),
                 reads=['desti', 'h2b%d' % j], writes=['xslots'], dma_key='disp%d' % ((2 * j + k) % 4))
    S.barrier(lambda e: e.memset(cx.scr, 0.0))
    M.release()
    if getattr(cx, 'moe_stop', None) == 'disp':
        M.release()
        return
    M.mark()
    NSTG = 10
    stgb = [M.alloc([8192], U8) for _ in range(NSTG)]
    stg = [a.bitcast(F32).rearrange("p (a b) -> p a b", a=4) for a in stgb]
    wg = [M.alloc([8, 512], BF16) for _ in range(3)]
    wu = [M.alloc([8, 512], BF16) for _ in range(3)]
    wd = [M.alloc([4, D], BF16) for _ in range(4)]
    Xb = [M.alloc([2, D], BF16) for _ in range(2)]
    XT = [M.alloc([8, BS], BF16) for _ in range(2)]
    AT = [M.alloc([4, BS], BF16) for _ in range(2)]
    sg = [M.alloc([BS], F32) for _ in range(2)]
    Ysb = [M.alloc([2, D], F32) for _ in range(2)]
    if 'ereg' not in cx.regs:
        cx.regs['ereg'] = None
    cnt3 = {'sidx': 0, 'g': 0, 'y': 0}
    NBL = getattr(cx, 'nb_limit', NB)

    def loadw(e, b, piece, sl):
        wt_h = (cx.w_gate, cx.w_up, cx.w_down)[piece // 2]
        wbt = wt_h.ap().bitcast(U8).tensor
        if cx.regs['ereg'] is None:
            cx.regs['ereg'] = st.enter_context(e.register("dynoff"))
        r = cx.regs['ereg']
        oc = (piece % 2) + (2 if piece >= 4 else 0)
        e.reg_load(r, offi[0:1, b, oc:oc + 1])
        v = e.snap(r, min_val=0, max_val=268435456 + 127 * 2097152 + 1048576)
        if piece < 4:
            src = bass.AP(wbt, v, [[16384, 128], [1, 8192]])
            ins = e.dma_start(out=stgb[sl], in_=src, bounds_check='skip_entire_dma')
        else:
            src = bass.AP(wbt, v, [[4096, 128], [128 * 4096, 2], [1, 4096]])
            ins = e.dma_start(out=stgb[sl].rearrange("p (c n) -> p c n", c=2), in_=src, bounds_check='skip_entire_dma')
        e.free_register(v.val)
        return ins

    def st_w(b):
        wbuf = b % 3
        w3 = b % 4
        for piece in range(6):
            sl = cnt3['sidx'] % NSTG
            cnt3['sidx'] += 1
            S.op('sync', lambda e, b=b, piece=piece, sl=sl: loadw(e, b, piece, sl), reads=['offi'], writes=['stg%d' % sl], dma_key='stg%d' % sl)
            if piece < 4:
                dstw = (wg if piece < 2 else wu)[wbuf]
                kk = 4 * (piece % 2)
                wkey = ('wg%d' if piece < 2 else 'wu%d') % wbuf
                if piece < 2:
                    S.op('vector', lambda e, dstw=dstw, kk=kk, sl=sl: e.tensor_copy(out=dstw[:, kk:kk + 4, :], in_=stg[sl]), reads=['stg%d' % sl], writes=[wkey])
                else:
                    S.op('scalar', lambda e, dstw=dstw, kk=kk, sl=sl: e.activation(out=dstw[:, kk:kk + 4, :], in_=stg[sl], func=AF.Copy), reads=['stg%d' % sl], writes=[wkey])
            else:
                cc = 2 * (piece - 4)
                srcv = stg[sl].rearrange("p a b -> p (a b)").rearrange("p (c n) -> p c n", c=2)
                if piece == 4:
                    S.op('vector', lambda e, cc=cc, srcv=srcv, w3=w3: e.tensor_copy(out=wd[w3][:, cc:cc + 2, :], in_=srcv), reads=['stg%d' % sl], writes=['wd%d' % w3])
                else:
                    S.op('scalar', lambda e, cc=cc, srcv=srcv, w3=w3: e.activation(out=wd[w3][:, cc:cc + 2, :], in_=srcv, func=AF.Copy), reads=['stg%d' % sl], writes=['wd%d' % w3])

    def st_t(b):
        xb = b % 2
        S.op('sync', lambda e, b=b, xb=xb: e.dma_start(out=Xb[xb], in_=cx.xslots[BS * b:BS * b + BS, :].rearrange("(s p) d -> p s d", p=128)), reads=['xslots'], writes=['Xb%d' % xb], dma_key='Xb%d' % xb)
        for sblk in range(2):
            pX = cx.psB[sblk]
            for k in range(8):
                S.op('tensor', lambda e, k=k, sblk=sblk, xb=xb, pX=pX: e.transpose(pX[:, 128 * k:128 * k + 128], Xb[xb][:, sblk, k:D:8], cx.identB), reads=['Xb%d' % xb, 'identB'], writes=['PS%d' % sblk])
            if sblk == 0:
                S.op('scalar', lambda e, xb=xb, pX=pX: e.activation(out=XT[xb][:, :, 0:128], in_=pX.rearrange("p (k t) -> p k t", k=8), func=AF.Copy), reads=['PS0'], writes=['XT%d' % xb])
            else:
                S.op('vector', lambda e, xb=xb, pX=pX: e.tensor_copy(out=XT[xb][:, :, 128:256], in_=pX.rearrange("p (k t) -> p k t", k=8)), reads=['PS1'], writes=['XT%d' % xb])

    def st_gu(b):
        xb = b % 2
        wbuf = b % 3
        for c in range(4):
            g2_ = cnt3['g'] % 2
            cnt3['g'] += 1
            pG = cx.psF[2 + g2_][:, 0:BS]
            pU = cx.psF[4 + g2_][:, 0:BS]
            for k in range(8):
                S.op('tensor', lambda e, k=k, c=c, xb=xb, wbuf=wbuf, pG=pG: e.matmul(pG, lhsT=wg[wbuf][:, k, 128 * c:128 * c + 128], rhs=XT[xb][:, k, :], start=(k == 0), stop=(k == 7)),
                     reads=['wg%d' % wbuf, 'XT%d' % xb], writes=['PS%d' % (2 + g2_)])
            S.op('scalar', lambda e, g2_=g2_, pG=pG: e.activation(out=sg[g2_], in_=pG, func=AF.Silu), reads=['PS%d' % (2 + g2_)], writes=['sg%d' % g2_])
            for k in range(8):
                S.op('tensor', lambda e, k=k, c=c, xb=xb, wbuf=wbuf, pU=pU: e.matmul(pU, lhsT=wu[wbuf][:, k, 128 * c:128 * c + 128], rhs=XT[xb][:, k, :], start=(k == 0), stop=(k == 7)),
                     reads=['wu%d' % wbuf, 'XT%d' % xb], writes=['PS%d' % (4 + g2_)])
            S.op('vector', lambda e, c=c, xb=xb, g2_=g2_, pU=pU: e.tensor_tensor(out=AT[xb][:, c, :], in0=pU, in1=sg[g2_], op=ALU.mult), reads=['PS%d' % (4 + g2_), 'sg%d' % g2_], writes=['AT%d' % xb])

    def st_y(b):
        xb = b % 2
        w3 = b % 4
        for sblk in range(2):
            for nh in range(2):
                pY = cx.psF[6 + (cnt3['y'] % 2)]
                pk = 'PS%d' % (6 + (cnt3['y'] % 2))
                cnt3['y'] += 1
                for c in range(4):
                    S.op('tensor', lambda e, c=c, sblk=sblk, nh=nh, xb=xb, w3=w3, pY=pY: e.matmul(pY[:, :], lhsT=AT[xb][:, c, 128 * sblk:128 * sblk + 128], rhs=wd[w3][:, c, 512 * nh:512 * nh + 512], start=(c == 0), stop=(c == 3)),
                         reads=['AT%d' % xb, 'wd%d' % w3], writes=[pk])
                if nh == 0:
                    S.op('scalar', lambda e, sblk=sblk, xb=xb, pY=pY: e.activation(out=Ysb[xb][:, sblk, 0:512], in_=pY[:, :], func=AF.Copy), reads=[pk], writes=['Ysb%d' % xb])
                else:
                    S.op('vector', lambda e, sblk=sblk, xb=xb, pY=pY: e.tensor_copy(out=Ysb[xb][:, sblk, 512:1024], in_=pY[:, :]), reads=[pk], writes=['Ysb%d' % xb])
        S.op('gpsimd', lambda e, b=b, xb=xb: e.dma_start(out=cx.yslots[BS * b:BS * b + BS, :].rearrange("(s p) d -> p s d", p=128), in_=Ysb[xb]), reads=['Ysb%d' % xb], writes=['yslots'], dma_key='yst%d' % xb)

    st_w(0)
    if NBL > 1:
        st_w(1)
    for it in range(NBL + 2):
        if it < NBL:
            st_t(it)
        if 0 <= it - 1 < NBL:
            st_gu(it - 1)
        if 0 <= it - 2 < NBL:
            st_y(it - 2)
        if it + 2 < NBL:
            st_w(it + 2)
    S.barrier(lambda e: e.memset(cx.scr, 0.0))
    M.release()
    if getattr(cx, 'moe_stop', None) == 'e3':
        M.release()
        return
    M.mark()
    gt4 = M.alloc([D], F32)
    Y1 = [M.alloc([D], F32) for _ in range(2)]
    Y2 = [M.alloc([D], F32) for _ in range(2)]
    xt4 = [M.alloc([D], F32) for _ in range(2)]
    xo4 = [M.alloc([D], F32) for _ in range(2)]
    yo4 = [M.alloc([D], F32) for _ in range(2)]
    load_mod_bc(S, cx, gt4, l, 5, 'gt2')
    if last:
        gf4 = M.alloc([D], F32)
        ss4 = M.alloc([NT], F32)
        rs4 = M.alloc([NT], F32)
        junk4 = M.alloc([D], BF16)
        S.op('sync', lambda e: e.dma_start(out=gf4, in_=_bc(cx.g_final.ap())), writes=['gf'], dma_key='gf')
    def e4_a(j):
        b = j % 2
        S.op('sync', lambda e, j=j, b=b: e.dma_start(out=xt4[b], in_=cx.moe_x[128 * j:128 * j + 128, :]), reads=['xres%d' % j], writes=['xt%d' % b], dma_key='xt%d' % b)
        S.op('gpsimd', lambda e, j=j, b=b: e.indirect_dma_start(out=Y1[b], out_offset=None, in_=cx.yslots[:, :], in_offset=bass.IndirectOffsetOnAxis(ap=desti[:, j, 0:1], axis=0),
                                                                bounds_check=_bnd(cx, e), oob_is_err=False), reads=['desti', 'yslots'], writes=['Y1_%d' % b], dma_key='Y1_%d' % b)
        S.op('gpsimd', lambda e, j=j, b=b: e.indirect_dma_start(out=Y2[b], out_offset=None, in_=cx.yslots[:, :], in_offset=bass.IndirectOffsetOnAxis(ap=desti[:, j, 1:2], axis=0),
                                                                bounds_check=_bnd(cx, e), oob_is_err=False), reads=['desti', 'yslots'], writes=['Y2_%d' % b], dma_key='Y2_%d' % b)

    def e4_b(j):
        b = j % 2
        S.op('vector', lambda e, j=j, b=b: e.tensor_scalar(out=Y1[b], in0=Y1[b], scalar1=wts[:, j, 0:1], scalar2=None, op0=ALU.mult), reads=['Y1_%d' % b, 'wts'], writes=['Y1_%d' % b])
        S.op('vector', lambda e, j=j, b=b: e.scalar_tensor_tensor(out=Y1[b], in0=Y2[b], scalar=wts[:, j, 1:2], in1=Y1[b], op0=ALU.mult, op1=ALU.add), reads=['Y1_%d' % b, 'Y2_%d' % b, 'wts'], writes=['Y1_%d' % b])
        S.op('gpsimd', lambda e, b=b: e.tensor_tensor(out=Y1[b], in0=Y1[b], in1=gt4, op=ALU.mult), reads=['Y1_%d' % b, 'gt2'], writes=['Y1_%d' % b])
        S.op('vector', lambda e, b=b: e.tensor_tensor(out=xo4[b], in0=xt4[b], in1=Y1[b], op=ALU.add), reads=['Y1_%d' % b, 'xt%d' % b], writes=['xo%d' % b])
        if not last:
            S.op('scalar', lambda e, j=j, b=b: e.dma_start(out=cx.xres[128 * j:128 * j + 128, :], in_=xo4[b]), reads=['xo%d' % b], writes=['xres%d' % j], dma_key='xst%d' % b)
        else:
            norm_mod_tile(S, cx, xo4[b], 'xo%d' % b, None, None, None, None, j, ss4, rs4, junk4)
            S.op('vector', lambda e, j=j, b=b: e.scalar_tensor_tensor(out=yo4[b], in0=xo4[b], scalar=rs4[:, j:j + 1], in1=gf4, op0=ALU.mult, op1=ALU.mult), reads=['xo%d' % b, 'rs%d' % j, 'gf'], writes=['yo%d' % b])
            o = S.op('scalar', lambda e, j=j, b=b: e.dma_start(out=cx.out_h[128 * j:128 * j + 128, :], in_=yo4[b]), reads=['yo%d' % b], writes=['out%d' % j], dma_key='ost%d' % b)
            S.final(o)
    e4_a(0)
    for j in range(NT):
        if j + 1 < NT:
            e4_a(j + 1)
        e4_b(j)
    S.barrier(lambda e: e.memset(cx.scr, 0.0))
    M.release()
    M.release()


def build_program(n_layers=DEPTH, dbg=False, phases=None, moe_stop=None, nb_limit=NB, static_w=False):
    nc = bass.Bass("TRN2", target_bir_lowering=False)
    cx = Ctx()
    cx.n_layers = n_layers
    cx.dbg = dbg
    cx.regs = {}
    cx.moe_stop = moe_stop
    cx.nb_limit = nb_limit
    cx.static_w = static_w
    L = DEPTH

    def din(name, shape, d=F32):
        return nc.dram_tensor(name, shape, d, kind="ExternalInput")
    cx.x_h = din("x", [S_LEN, D])
    cx.c_h = din("c", [1, D])
    cx.w_mod = din("w_mod", [L, D, 6 * D])
    cx.b_mod = din("b_mod", [L, 6 * D])
    cx.g_norm1 = din("g_norm1", [L, D])
    cx.w_in = din("w_in", [L, D, DIN])
    cx.conv_pack = din("conv_pack", [L, 34, 512])
    cx.w_out = din("w_out", [L, D, D])
    cx.g_norm2 = din("g_norm2", [L, D])
    cx.w_r = din("w_r", [L, D, 36])
    cx.b_r = din("b_r", [L, 36])
    cx.w_gate = din("w_exp_gate", [L, NE, D, DE])
    cx.w_up = din("w_exp_up", [L, NE, D, DE])
    cx.w_down = din("w_exp_down", [L, NE, DE, D])
    cx.g_final = din("g_final", [1, D])
    cx.cf32 = din("cf32", [128, 768 + NB * 32])
    cx.cbf = din("cbf", [128, 384], BF16)
    cx.wtab_h = din("wtab", [128, NH * 3 * 256], BF16)
    cx.out_h = nc.dram_tensor("out", [S_LEN, D], F32, kind="ExternalOutput")
    skind = "ExternalOutput" if dbg else "Internal"
    cx.xres = nc.dram_tensor("xres", [S_LEN, D], F32, kind=skind)
    cx.mixT = nc.dram_tensor("mixT", [D, S_LEN], BF16, kind=skind)
    cx.modscr = nc.dram_tensor("modscr", [L, 6 * D], F32, kind=skind)
    cx.xslots = nc.dram_tensor("xslots", [NSLOT, D], BF16, kind="Internal")
    cx.yslots = nc.dram_tensor("yslots", [NSLOT, D], F32, kind="Internal")
    if dbg:
        cx.dbg_lg = nc.dram_tensor("dbg_lg", [S_LEN, 36], F32, kind="ExternalOutput")
        cx.dbg_dest = nc.dram_tensor("dbg_dest", [S_LEN, 2], I32, kind="ExternalOutput")
        cx.dbg_wts = nc.dram_tensor("dbg_wts", [S_LEN, 2], F32, kind="ExternalOutput")
        cx.dbg_eb = nc.dram_tensor("dbg_eb", [1, 64], I32, kind="ExternalOutput")
        cx.dbg_h2 = nc.dram_tensor("dbg_h2", [S_LEN, D], BF16, kind="ExternalOutput")
        cx.dbg_rs = nc.dram_tensor("dbg_rs", [128, NT], F32, kind="ExternalOutput")
        cx.dbg_hT = nc.dram_tensor("dbg_hT", [128, 8 * S_LEN], BF16, kind="ExternalOutput")
    with ExitStack() as st:
        cx.stack = st
        pool = st.enter_context(nc.sbuf_tensor("pool", [128, POOLB], dt.uint8))
        cx.mem = Mem(pool, POOLB)
        cx.psF = [st.enter_context(nc.psum_tensor("psb%d" % i, [128, 512], F32)) for i in range(8)]
        cx.psB = [p[:, :].bitcast(BF16) for p in cx.psF]
        cx.psF = [p[:, :] for p in cx.psF]
        S = Sched(nc)
        cx.epsT = cx.mem.alloc([1], F32)
        S.op('vector', lambda e: e.memset(cx.epsT, EPS), writes=['epsT'])
        phase_prologue(S, cx)
        cx.hT = None
        for l in range(n_layers):
            last = (l == n_layers - 1)
            cx.mem.mark()
            cx.hT = cx.mem.alloc([8, S_LEN], BF16)
            if phases is None or 'm1' in phases:
                phase_m1(S, cx, l)
                if dbg and l == 0:
                    S.op('sync', lambda e: e.dma_start(out=cx.dbg_hT.ap(), in_=cx.hT.rearrange("p k t -> p (k t)")), reads=['hT'], writes=['dbg_hT'], dma_key='dbg0')
            if phases is None or 'm2' in phases:
                phase_m2(S, cx, l)
            if phases is None or 'm3' in phases:
                phase_m3(S, cx, l)
            if phases is None or 'm4' in phases:
                phase_m4(S, cx, l)
            cx.mem.release()
            cx.moe_x = cx.xres if (phases is None or 'm4' in phases or l > 0) else cx.x_h
            if phases is None or 'moe' in phases:
                phase_moe(S, cx, l, last)
        for k, o in list(S.last_w.items()):
            if isinstance(k, str) and k.startswith('dbg'):
                S.final(o)
        endop = S.barrier(lambda e: e.memset(cx.scr, 0.0))
        S.final(endop)
        S.emit(st)
        cx.n_ops = S.nops
        cx.peak = cx.mem.peak
    return nc, cx


def host_consts():
    cf = np.zeros((128, 768 + NB * 32), np.float32)
    cf[:, 0:128] = np.eye(128, dtype=np.float32)
    cf[:, 128:256] = 1.0 / 512.0
    th = (np.arange(16, dtype=np.float32) * BS)[None, :].repeat(32, 0)
    cf[:, 256:768] = th.reshape(-1)[None, :]
    bi = np.arange(NB, dtype=np.float32)[:, None].repeat(32, 1)
    cf[:, 768:] = bi.reshape(-1)[None, :]
    cb = np.zeros((128, 384), np.float32)
    cb[:, 0:128] = np.eye(128)
    cb[:, 128:256] = 1.0
    pp = np.arange(128)
    cb[:, 256:384] = (pp[:, None] < pp[None, :]).astype(np.float32)
    cb = cb.astype(ml_dtypes.bfloat16)
    slopes = 2.0 ** (-8.0 * np.arange(1, NH + 1) / NH)
    wt = np.zeros((128, NH, 3, 256), np.float64)
    ki = np.arange(128)[:, None]
    for bi_, dil in enumerate(DILS):
        for h in range(NH):
            qi = np.arange(128)[None, :]
            d_prev = 128 + qi - ki
            v_prev = (d_prev >= 0) & (d_prev <= 128)
            d_cur = qi - ki
            v_cur = (d_cur >= 0) & (d_cur <= 128)
            wt[:, h, bi_, 0:128] = np.where(v_prev, np.exp(-slopes[h] * dil * np.where(v_prev, d_prev, 0)), 0.0)
            wt[:, h, bi_, 128:256] = np.where(v_cur, np.exp(-slopes[h] * dil * np.where(v_cur, d_cur, 0)), 0.0)
    return cf, cb, wt.reshape(128, -1).astype(np.float32).astype(ml_dtypes.bfloat16)


_CACHE = {}


def make_in_maps(inputs, n_cores=8):
    f = lambda a: np.ascontiguousarray(np.asarray(a, dtype=np.float32))
    cf, cb, wt = host_consts()
    conv_pack = np.concatenate([f(inputs['conv_w']), f(inputs['conv_b'])[:, None, :], f(inputs['conv_ln_g'])[:, None, :], f(inputs['conv_ln_b'])[:, None, :]], axis=1)
    w_r = np.concatenate([f(inputs['w_router_group']), f(inputs['w_router_expert'])], axis=2)
    b_r = np.concatenate([f(inputs['b_router_group']), f(inputs['b_router_expert'])], axis=1)
    shared = {
        "w_mod": f(inputs['w_mod']), "b_mod": f(inputs['b_mod']), "g_norm1": f(inputs['g_norm1']), "w_in": f(inputs['w_in']),
        "conv_pack": np.ascontiguousarray(conv_pack), "w_out": f(inputs['w_out']), "g_norm2": f(inputs['g_norm2']),
        "w_r": np.ascontiguousarray(w_r), "b_r": np.ascontiguousarray(b_r),
        "w_exp_gate": f(inputs['w_exp_gate']), "w_exp_up": f(inputs['w_exp_up']), "w_exp_down": f(inputs['w_exp_down']),
        "g_final": f(inputs['g_final']).reshape(1, D), "cf32": cf, "cbf": cb, "wtab": wt,
    }
    x = f(inputs['x'])
    c = f(inputs['c'])
    maps = []
    for b in range(n_cores):
        m = dict(shared)
        m["x"] = np.ascontiguousarray(x[b])
        m["c"] = np.ascontiguousarray(c[b:b + 1])
        maps.append(m)
    return maps


def kernel(**inputs):
    if 'nc' not in _CACHE:
        _CACHE['nc'] = build_program(DEPTH, dbg=False)[0]
    nc = _CACHE['nc']
    maps = make_in_maps(inputs, 8)
    res = run_bass_kernel_spmd(nc, maps, core_ids=list(range(8)))
    out = np.stack([np.asarray(res.results[b]["out"], dtype=np.float32) for b in range(8)], axis=0)
    return out
```

```python
import numpy as np
import ml_dtypes
from contextlib import ExitStack
import concourse.bass as bass
import concourse.mybir as mybir
from concourse.bass_utils import run_bass_kernel_spmd

dt = mybir.dt
AF = mybir.ActivationFunctionType
ALU = mybir.AluOpType
AX = mybir.AxisListType
F32 = dt.float32
BF16 = dt.bfloat16
I32 = dt.int32

S_LEN = 4096
D = 1024
NT = 32
DEPTH = 4
DIN = 2560
NH = 8
NE = 32
DE = 512
BS = 256
NB = 63
NSLOT = NB * BS
EPS = 1e-6
BIG = 1.0e30
DILS = (1, 4, 16)
POOLB = 206 * 1024

ENGS = ('sync', 'scalar', 'vector', 'gpsimd', 'tensor')
U8 = dt.uint8
_SZ = {F32: 4, BF16: 2, I32: 4, U8: 1}


class Op:
    __slots__ = ('eng', 'fn', 'deps', 'dma_key', 'ninst', 'has_dep', 'sigval', 'sem', 'idx')


class Sched:
    def __init__(self, nc):
        self.nc = nc
        self.ops = {e: [] for e in ENGS}
        self.last_w = {}
        self.readers = {}
        self.finals = []
        self.nops = 0
        self.since = []
        self.bar = None

    def op(self, eng, fn, reads=(), writes=(), dma_key=None, ninst=1, extra=()):
        o = Op()
        o.eng = eng
        o.fn = fn
        o.dma_key = dma_key
        o.ninst = ninst
        o.has_dep = False
        o.sigval = None
        o.sem = None
        o.idx = self.nops
        self.nops += 1
        ex = [r for r in reads if isinstance(r, str) and r.startswith('PS')]
        if ex:
            reads = [r for r in reads if r not in ex]
            writes = list(writes) + ex
        deps = {}
        raw = set()
        for r in reads:
            w = self.last_w.get(r)
            if w is not None:
                deps[id(w)] = w
                raw.add(id(w))
        for w_ in writes:
            w = self.last_w.get(w_)
            if w is not None:
                deps[id(w)] = w
            for rd in self.readers.get(w_, ()):
                deps[id(rd)] = rd
        for d in extra:
            deps[id(d)] = d
        if self.bar is not None:
            deps[id(self.bar)] = self.bar
        keep = []
        for d in deps.values():
            if d is o:
                continue
            if d.dma_key is None and d.eng == eng and dma_key is None:
                if eng == 'tensor' or id(d) not in raw:
                    continue
            keep.append(d)
            d.has_dep = True
        o.deps = keep
        for r in reads:
            self.readers.setdefault(r, []).append(o)
        for w_ in writes:
            self.last_w[w_] = o
            self.readers[w_] = []
        self.ops[eng].append(o)
        self.since.append(o)
        return o

    def barrier(self, fn, eng='vector'):
        last = {}
        for o in self.since:
            key = ('d', o.dma_key) if o.dma_key is not None else ('e', o.eng)
            last[key] = o
        prev = list(last.values())
        self.since = []
        self.last_w = {}
        self.readers = {}
        o = self.op(eng, fn, extra=prev)
        self.bar = o
        return o

    def final(self, o):
        o.has_dep = True
        self.finals.append(o)

    def emit(self, stack):
        nc = self.nc
        esem = {e: stack.enter_context(nc.semaphore("es_" + e)) for e in ENGS}
        dsem = {}
        dcount = {}
        for e in ENGS:
            cnt = 0
            for o in self.ops[e]:
                if o.dma_key is None and o.has_dep:
                    cnt += 1
                    o.sigval = cnt
                    o.sem = esem[e]
        dmaops = sorted([o for e in ENGS for o in self.ops[e] if o.dma_key is not None], key=lambda o: o.idx)
        for o in dmaops:
            k = o.dma_key
            if k not in dsem:
                dsem[k] = stack.enter_context(nc.semaphore("ds_" + str(k)))
                dcount[k] = 0
            dcount[k] += 16 * o.ninst
            o.sigval = dcount[k]
            o.sem = dsem[k]
        self.n_sems = len(esem) + len(dsem)
        block = stack.enter_context(nc.Block())
        finals = self.finals

        def run_engine(e, eng):
            waited = {}
            for o in self.ops[e]:
                need = {}
                for d in o.deps:
                    key = id(d.sem)
                    if key not in need or need[key][1] < d.sigval:
                        need[key] = (d.sem, d.sigval)
                for key, (s, v) in need.items():
                    if waited.get(key, 0) >= v:
                        continue
                    eng.wait_ge(s, v)
                    waited[key] = v
                r = o.fn(eng)
                if o.dma_key is not None:
                    ins = r if isinstance(r, (list, tuple)) else [r]
                    assert len(ins) == o.ninst, (len(ins), o.ninst)
                    for i_ in ins:
                        i_.then_inc(o.sem, 16)
                elif o.has_dep:
                    ins = r[-1] if isinstance(r, (list, tuple)) else r
                    ins.then_inc(o.sem, 1)
            if e == 'sync':
                need = {}
                for d in finals:
                    key = id(d.sem)
                    if key not in need or need[key][1] < d.sigval:
                        need[key] = (d.sem, d.sigval)
                for key, (s, v) in need.items():
                    eng.wait_ge(s, v)

        @block.sync
        def _(eng):
            run_engine('sync', eng)

        @block.scalar
        def _(eng):
            run_engine('scalar', eng)

        @block.vector
        def _(eng):
            run_engine('vector', eng)

        @block.gpsimd
        def _(eng):
            run_engine('gpsimd', eng)

        @block.tensor
        def _(eng):
            run_engine('tensor', eng)


class Mem:
    def __init__(self, pool, size):
        self.pool = pool
        self.size = size
        self.top = 0
        self.marks = []
        self.peak = 0

    def alloc(self, shape, dtype, p0=0, p1=128):
        n = 1
        for s in shape:
            n *= s
        nb = n * _SZ[dtype]
        off = (self.top + 31) // 32 * 32
        self.top = off + nb
        self.peak = max(self.peak, self.top)
        assert self.top <= self.size, ("SBUF pool overflow", self.top, self.size)
        ap = self.pool[p0:p1, off:off + nb]
        if dtype != U8:
            ap = ap.bitcast(dtype)
        if len(shape) == 2:
            ap = ap.rearrange("p (a b) -> p a b", a=shape[0])
        elif len(shape) == 3:
            ap = ap.rearrange("p (a b c) -> p a b c", a=shape[0], b=shape[1])
        elif len(shape) == 4:
            ap = ap.rearrange("p (a b c d) -> p a b c d", a=shape[0], b=shape[1], c=shape[2])
        return ap

    def mark(self):
        self.marks.append(self.top)

    def release(self):
        self.top = self.marks.pop()


class Ctx:
    pass


def _bnd(cx, e):
    if cx.regs.get('bnd') is None:
        cx.regs['bnd'] = e.to_reg(NSLOT - 1)
    return cx.regs['bnd']


def _bc(ap_row):
    b = ap_row.partition_broadcast(128)
    if len(b.shape) == 3:
        b = b.rearrange("p o n -> p (o n)")
    return b


def phase_prologue(S, cx):
    M = cx.mem
    cx.identF = M.alloc([128], F32)
    cx.identB = M.alloc([128], BF16)
    cx.onesB = M.alloc([128], BF16)
    cx.rstrictB = M.alloc([128], BF16)
    cx.onesD = M.alloc([128], F32)
    cx.scr = M.alloc([8], F32)
    cx.scr2 = M.alloc([8], F32)
    S.op('gpsimd', lambda e: e.memset(cx.scr2, 0.0), writes=['scr2'])
    S.op('sync', lambda e: e.dma_start(out=cx.identF, in_=cx.cf32[:, 0:128]), writes=['identF'], dma_key='c0')
    S.op('sync', lambda e: e.dma_start(out=cx.onesD, in_=cx.cf32[:, 128:256]), writes=['onesD'], dma_key='c1')
    S.op('sync', lambda e: e.dma_start(out=cx.identB, in_=cx.cbf[:, 0:128]), writes=['identB'], dma_key='c2')
    S.op('sync', lambda e: e.dma_start(out=cx.onesB, in_=cx.cbf[:, 128:256]), writes=['onesB'], dma_key='c3')
    S.op('sync', lambda e: e.dma_start(out=cx.rstrictB, in_=cx.cbf[:, 256:384]), writes=['rstrictB'], dma_key='c4')
    M.mark()
    cc = M.alloc([8], F32)
    cact = M.alloc([8], F32)
    cbc = M.alloc([8, 128], F32)
    bmod = M.alloc([6 * D], F32, 0, 1)
    modrow = M.alloc([6 * D], F32, 0, 1)
    stage = [M.alloc([8, 512], F32) for _ in range(2)]
    S.op('sync', lambda e: e.dma_start(out=cc, in_=cx.c_h.ap().rearrange("o (p k) -> (o p) k", k=8)), writes=['cc'], dma_key='c5')
    S.op('scalar', lambda e: e.activation(out=cact, in_=cc, func=AF.Silu), reads=['cc'], writes=['cact'])
    S.op('vector', lambda e: e.tensor_copy(out=cbc, in_=cact.unsqueeze(2).to_broadcast([128, 8, 128])), reads=['cact'], writes=['cbc'])
    it = 0
    for l in range(cx.n_layers):
        S.op('sync', lambda e, l=l: e.dma_start(out=bmod, in_=cx.b_mod[l:l + 1, :]), writes=['bmod'], dma_key='bm')
        for n in range(12):
            sl = it % 2
            it += 1
            src = cx.w_mod[l].rearrange("(p k) n -> p k n", k=8)[:, :, 512 * n:512 * n + 512]
            S.op('sync', lambda e, sl=sl, src=src: e.dma_start(out=stage[sl], in_=src), writes=['pst%d' % sl], dma_key='pst%d' % sl)
            ps = cx.psF[sl]
            for k in range(8):
                S.op('tensor', lambda e, k=k, sl=sl, ps=ps: e.matmul(ps[:, :], lhsT=cbc[:, k, :], rhs=stage[sl][:, k, :], start=(k == 0), stop=(k == 7)),
                     reads=['cbc', 'pst%d' % sl], writes=['PS%d' % sl])
            S.op('vector', lambda e, n=n, ps=ps: e.tensor_tensor(out=modrow[0:1, 512 * n:512 * n + 512], in0=ps[0:1, :], in1=bmod[0:1, 512 * n:512 * n + 512], op=ALU.add),
                 reads=['PS%d' % sl, 'bmod'], writes=['modrow'])
        S.op('sync', lambda e, l=l: e.dma_start(out=cx.modscr[l:l + 1, :], in_=modrow[0:1, :]), reads=['modrow'], writes=['modscr%d' % l], dma_key='ms')
    S.barrier(lambda e: e.memset(cx.scr, 0.0))
    M.release()


def load_mod_bc(S, cx, dst, l, idx, key):
    S.op('sync', lambda e: e.dma_start(out=dst, in_=_bc(cx.modscr[l:l + 1, idx * D:(idx + 1) * D])), writes=[key], dma_key='mb_' + key)


def norm_mod_tile(S, cx, xt, xkey, gp, sh, hb, hkey, j, ss, rs, junk, out_dt_note=''):
    def sq(e):
        i1 = e.activation(out=junk, in_=xt, func=AF.Square, accum_out=ss[:, j:j + 1])
        i2 = e.activation(out=cx.scr2[:, 0:1], in_=cx.scr2[:, 1:2], func=AF.Copy)
        return [i1, i2]
    S.op('scalar', sq, reads=[xkey], writes=['junk', 'ss%d' % j])
    S.op('scalar', lambda e: e.activation(out=rs[:, j:j + 1], in_=ss[:, j:j + 1], func=AF.Sqrt, bias=cx.epsT[:, 0:1], scale=1.0 / D), reads=['ss%d' % j], writes=['rs%d' % j])
    S.op('vector', lambda e: e.reciprocal(out=rs[:, j:j + 1], in_=rs[:, j:j + 1]), reads=['rs%d' % j], writes=['rs%d' % j])


def phase_m1(S, cx, l):
    M = cx.mem
    M.mark()
    g1 = M.alloc([D], F32)
    sc = M.alloc([D], F32)
    sh = M.alloc([D], F32)
    xt = [M.alloc([D], F32) for _ in range(2)]
    tmp = [M.alloc([D], F32) for _ in range(2)]
    hb = [M.alloc([D], BF16) for _ in range(2)]
    junk = M.alloc([D], BF16)
    ss = M.alloc([NT], F32)
    rs = M.alloc([NT], F32)
    hT = cx.hT
    S.op('sync', lambda e: e.dma_start(out=g1, in_=_bc(cx.g_norm1[l:l + 1, :])), writes=['g1'], dma_key='m1g')
    load_mod_bc(S, cx, sh, l, 0, 'sh1')
    load_mod_bc(S, cx, sc, l, 1, 'sc1')
    S.op('vector', lambda e: e.scalar_tensor_tensor(out=g1, in0=sc, scalar=1.0, in1=g1, op0=ALU.add, op1=ALU.mult), reads=['sc1', 'g1'], writes=['g1'])
    xsrc = cx.x_h if l == 0 else cx.xres

    def stage_a(j):
        b = j % 2
        S.op('sync', lambda e, j=j, b=b: e.dma_start(out=xt[b], in_=xsrc[128 * j:128 * j + 128, :]), reads=['xres'], writes=['xt%d' % b], dma_key='xt%d' % b)
        norm_mod_tile(S, cx, xt[b], 'xt%d' % b, g1, sh, None, None, j, ss, rs, junk)
        S.op('vector', lambda e, j=j, b=b: e.scalar_tensor_tensor(out=tmp[b], in0=xt[b], scalar=rs[:, j:j + 1], in1=g1, op0=ALU.mult, op1=ALU.mult),
             reads=['xt%d' % b, 'rs%d' % j, 'g1'], writes=['tmp%d' % b])
        S.op('gpsimd', lambda e, b=b: e.tensor_tensor(out=hb[b], in0=tmp[b], in1=sh, op=ALU.add), reads=['tmp%d' % b, 'sh1'], writes=['hb%d' % b])

    def stage_b(j):
        b = j % 2
        pT = cx.psB[b]
        for k in range(8):
            S.op('tensor', lambda e, k=k, b=b, pT=pT: e.transpose(pT[:, 128 * k:128 * k + 128], hb[b][:, k:D:8], cx.identB), reads=['hb%d' % b, 'identB'], writes=['PS%d' % b])
        S.op('scalar', lambda e, j=j, pT=pT: e.activation(out=hT[:, :, 128 * j:128 * j + 128], in_=pT.rearrange("p (k t) -> p k t", k=8), func=AF.Copy),
             reads=['PS%d' % b], writes=['hT'])
    stage_a(0)
    for j in range(NT):
        if j + 1 < NT:
            stage_a(j + 1)
        stage_b(j)
    S.barrier(lambda e: e.memset(cx.scr, 0.0))
    M.release()


def load_win_chunk(S, cx, l, oc, stg, skey, wbf, wkey):
    src = cx.w_in[l].rearrange("(p k) n -> p k n", k=8)[:, :, 128 * oc:128 * oc + 128]
    S.op('sync', lambda e: e.dma_start(out=stg, in_=src), writes=[skey], dma_key='ld_' + skey)
    S.op('gpsimd', lambda e: e.tensor_copy(out=wbf, in_=stg), reads=[skey], writes=[wkey])


def phase_m2(S, cx, l):
    M = cx.mem
    M.mark()
    hT = cx.hT
    uT = M.alloc([4, 30 + S_LEN], BF16)
    stg = [M.alloc([8, 128], F32) for _ in range(2)]
    wa = [M.alloc([8, 128], BF16) for _ in range(2)]
    wb = [M.alloc([8, 128], BF16) for _ in range(2)]
    sig = [M.alloc([512], F32) for _ in range(2)]
    cpk = M.alloc([512], F32, 0, 34)
    cw = M.alloc([4, 34], F32)
    for c in range(4):
        S.op('gpsimd', lambda e, c=c: e.memset(uT[:, c, 0:30], 0.0), writes=['uT%d' % c])
    S.op('sync', lambda e: e.dma_start(out=cpk[0:34, :], in_=cx.conv_pack[l]), writes=['cpk'], dma_key='cpk')
    pc = cx.psF[6]
    for c in range(4):
        S.op('tensor', lambda e, c=c: e.transpose(pc[:, 34 * c:34 * c + 34], cpk[0:34, 128 * c:128 * c + 128], cx.identF[0:34, 0:34]), reads=['cpk', 'identF'], writes=['PS6'])
    S.op('vector', lambda e: e.tensor_copy(out=cw, in_=pc[:, 0:136].rearrange("p (c r) -> p c r", c=4)), reads=['PS6'], writes=['cw'])
    it = 0
    for c in range(4):
        b = c % 2
        load_win_chunk(S, cx, l, 12 + c, stg[0], 'stgA', wa[b], 'wa%d' % b)
        load_win_chunk(S, cx, l, 16 + c, stg[1], 'stgB', wb[b], 'wb%d' % b)
        for tb in range(8):
            s2 = it % 2
            it += 1
            pa = cx.psF[s2]
            pb = cx.psF[2 + s2]
            for k in range(8):
                S.op('tensor', lambda e, k=k, b=b, tb=tb, pa=pa: e.matmul(pa[:, :], lhsT=wa[b][:, k, :], rhs=hT[:, k, 512 * tb:512 * tb + 512], start=(k == 0), stop=(k == 7)),
                     reads=['wa%d' % b, 'hT'], writes=['PS%d' % s2])
            for k in range(8):
                S.op('tensor', lambda e, k=k, b=b, tb=tb, pb=pb: e.matmul(pb[:, :], lhsT=wb[b][:, k, :], rhs=hT[:, k, 512 * tb:512 * tb + 512], start=(k == 0), stop=(k == 7)),
                     reads=['wb%d' % b, 'hT'], writes=['PS%d' % (2 + s2)])
            S.op('scalar', lambda e, s2=s2, pb=pb: e.activation(out=sig[s2], in_=pb[:, :], func=AF.Sigmoid), reads=['PS%d' % (2 + s2)], writes=['sig%d' % s2])
            S.op('vector', lambda e, c=c, tb=tb, s2=s2, pa=pa: e.tensor_tensor(out=uT[:, c, 30 + 512 * tb:30 + 512 * tb + 512], in0=pa[:, :], in1=sig[s2], op=ALU.mult),
                 reads=['PS%d' % s2, 'sig%d' % s2], writes=['uT%d' % c])
    dg = M.alloc([4, 31, 128], BF16)
    for c in range(4):
        for j in range(31):
            eng = 'vector' if (j % 2 == 0) else 'gpsimd'
            S.op(eng, lambda e, c=c, j=j: e.tensor_scalar(out=dg[:, c, j, :], in0=cx.identB, scalar1=cw[:, c, j:j + 1], scalar2=None, op0=ALU.mult),
                 reads=['identB', 'cw'], writes=['dg%d_%d' % (c, j)])
    ysb = M.alloc([4, 512], F32)
    y2 = M.alloc([4, 512], F32)
    mean = M.alloc([512], F32)
    var = M.alloc([512], F32)
    tz = [M.alloc([512], F32) for _ in range(2)]
    cv = [M.alloc([4, 512], BF16) for _ in range(2)]
    mixT_v = cx.mixT.rearrange("(k p) t -> p k t", p=128)
    for tb in range(8):
        for c in range(4):
            py = cx.psF[c]
            for j in range(31):
                S.op('tensor', lambda e, c=c, j=j, tb=tb, py=py: e.matmul(py[:, :], lhsT=dg[:, c, j, :], rhs=uT[:, c, 512 * tb + j:512 * tb + j + 512], start=(j == 0), stop=(j == 30)),
                     reads=['dg%d_%d' % (c, j), 'uT%d' % c], writes=['PS%d' % c])
            S.op('scalar', lambda e, c=c, py=py: e.activation(out=ysb[:, c, :], in_=py[:, :], func=AF.Identity, bias=cw[:, c, 31:32]), reads=['PS%d' % c, 'cw'], writes=['ysb%d' % c])
            S.op('scalar', lambda e, c=c, py=py: e.activation(out=y2[:, c, :], in_=py[:, :], func=AF.Square, bias=cw[:, c, 31:32]), reads=['PS%d' % c, 'cw'], writes=['y2%d' % c])
        pm = cx.psF[4]
        pq = cx.psF[5]
        for c in range(4):
            S.op('tensor', lambda e, c=c: e.matmul(pm[:, :], lhsT=cx.onesD, rhs=ysb[:, c, :], start=(c == 0), stop=(c == 3)), reads=['onesD', 'ysb%d' % c], writes=['PS4'])
        for c in range(4):
            S.op('tensor', lambda e, c=c: e.matmul(pq[:, :], lhsT=cx.onesD, rhs=y2[:, c, :], start=(c == 0), stop=(c == 3)), reads=['onesD', 'y2%d' % c], writes=['PS5'])
        S.op('scalar', lambda e: e.activation(out=mean, in_=pm[:, :], func=AF.Copy), reads=['PS4'], writes=['mean'])
        S.op('vector', lambda e: e.tensor_tensor(out=var, in0=pm[:, :], in1=mean, op=ALU.mult), reads=['PS4', 'mean'], writes=['var'])
        S.op('vector', lambda e: e.tensor_tensor(out=var, in0=pq[:, :], in1=var, op=ALU.subtract), reads=['PS5', 'var'], writes=['var'])
        S.op('scalar', lambda e: e.activation(out=var, in_=var, func=AF.Sqrt, bias=cx.epsT[:, 0:1], scale=1.0), reads=['var'], writes=['var'])
        S.op('vector', lambda e: e.reciprocal(out=var, in_=var), reads=['var'], writes=['var'])
        cb = tb % 2
        for c in range(4):
            z = tz[c % 2]
            zk = 'tz%d' % (c % 2)
            S.op('vector', lambda e, c=c, z=z: e.tensor_tensor(out=z, in0=ysb[:, c, :], in1=mean, op=ALU.subtract), reads=['ysb%d' % c, 'mean'], writes=[zk])
            S.op('vector', lambda e, z=z: e.tensor_tensor(out=z, in0=z, in1=var, op=ALU.mult), reads=[zk, 'var'], writes=[zk])
            S.op('scalar', lambda e, c=c, z=z, cb=cb: e.activation(out=cv[cb][:, c, :], in_=z, func=AF.Silu, bias=cw[:, c, 33:34], scale=cw[:, c, 32:33]), reads=[zk, 'cw'], writes=['cv%d' % cb])
        S.op('gpsimd', lambda e, tb=tb, cb=cb: e.dma_start(out=mixT_v[:, 4:8, 512 * tb:512 * tb + 512], in_=cv[cb]), reads=['cv%d' % cb], writes=['mixT'], dma_key='cvst%d' % cb)
    S.barrier(lambda e: e.memset(cx.scr, 0.0))
    M.release()


def qblocks(dil, half):
    nbr = 32 // dil
    hb = nbr // 2
    return [(r, n) for r in range(dil) for n in range(half * hb, half * hb + hb)]


def phase_m3(S, cx, l):
    M = cx.mem
    M.mark()
    hT = cx.hT
    wtab = M.alloc([NH, 3, 256], BF16)
    QT = M.alloc([S_LEN], BF16)
    KT = M.alloc([S_LEN], BF16)
    VT = M.alloc([S_LEN], BF16)
    Vt = M.alloc([3 * 32 * 256], BF16)
    acc = [M.alloc([2048], F32) for _ in range(2)]
    Et = [M.alloc([256], F32) for _ in range(4)]
    Pt = [M.alloc([256], BF16) for _ in range(4)]
    rd = M.alloc([2048], F32)
    aT = [M.alloc([2048], BF16) for _ in range(2)]
    stg = [M.alloc([8, 128], F32) for _ in range(2)]
    wq = [M.alloc([8, 128], BF16) for _ in range(3)]
    S.op('sync', lambda e: e.dma_start(out=wtab, in_=cx.wtab_h.ap().rearrange("p (h b c) -> p h b c", h=NH, b=3)), writes=['wtab'], dma_key='wtab')
    S.op('gpsimd', lambda e: e.memset(Vt, 1.0), writes=['Vt'])
    mixT_v = cx.mixT.rearrange("(k p) t -> p k t", p=128)
    pcount = 0
    ecount = 0
    ocount = 0
    for hp in range(4):
        for wi, (oc, dst, name) in enumerate(((hp, QT, 'QT'), (4 + hp, KT, 'KT'), (8 + hp, VT, 'VT'))):
            load_win_chunk(S, cx, l, oc, stg[wi % 2], 'stg%d' % (wi % 2), wq[wi], 'wq%d' % wi)
            for tb in range(8):
                s2 = pcount % 2
                pcount += 1
                pp = cx.psF[s2]
                for k in range(8):
                    S.op('tensor', lambda e, k=k, wi=wi, tb=tb, pp=pp: e.matmul(pp[:, :], lhsT=wq[wi][:, k, :], rhs=hT[:, k, 512 * tb:512 * tb + 512], start=(k == 0), stop=(k == 7)),
                         reads=['wq%d' % wi, 'hT'], writes=['PS%d' % s2])
                if name == 'QT':
                    S.op('scalar', lambda e, tb=tb, pp=pp, dst=dst: e.activation(out=dst[:, 512 * tb:512 * tb + 512], in_=pp[:, :], func=AF.Copy, scale=0.125),
                         reads=['PS%d' % s2], writes=[name])
                else:
                    S.op('vector', lambda e, tb=tb, pp=pp, dst=dst: e.tensor_copy(out=dst[:, 512 * tb:512 * tb + 512], in_=pp[:, :]), reads=['PS%d' % s2], writes=[name])
        for di, dil in enumerate(DILS):
            nbr = 32 // dil
            for g in range(8):
                pv = cx.psB[g % 2]
                for q4 in range(4):
                    blk = 4 * g + q4
                    r, n = blk // nbr, blk % nbr
                    t0 = r + dil * 128 * n
                    S.op('tensor', lambda e, q4=q4, t0=t0, dil=dil, pv=pv: e.transpose(pv[:, 128 * q4:128 * q4 + 128], VT[:, t0:t0 + dil * 127 + 1:dil], cx.identB),
                         reads=['VT', 'identB'], writes=['PS%d' % (g % 2)])
                vdst = bass.AP(Vt.tensor, Vt.offset + di * 8192 + g * 1024, [list(Vt.ap[0]), [256, 4], [192, 2], [1, 64]])
                vsrc = pv[:, 0:512].rearrange("p (q s d) -> p q s d", q=4, s=2)
                S.op('scalar' if g % 2 == 0 else 'vector',
                     (lambda e, vdst=vdst, vsrc=vsrc: e.activation(out=vdst, in_=vsrc, func=AF.Copy)) if g % 2 == 0 else
                     (lambda e, vdst=vdst, vsrc=vsrc: e.tensor_copy(out=vdst, in_=vsrc)),
                     reads=['PS%d' % (g % 2)], writes=['Vt'])
        for s in range(2):
            h = 2 * hp + s
            p0, p1 = 64 * s, 64 * s + 64
            for half in range(2):
                ab = (2 * s + half) % 2
                A = acc[ab]
                akey = 'acc%d' % ab
                blist = [(di, dil, r, n) for di, dil in enumerate(DILS) for (r, n) in qblocks(dil, half)]
                LAG = 3
                pend = {}
                for t in range(len(blist) + LAG):
                    if t < len(blist):
                        di, dil, r, n = blist[t]
                        nbr = 32 // dil
                        has_prev = n > 0
                        lo = 0 if has_prev else 128
                        tq = r + dil * 128 * n
                        qs = QT[p0:p1, tq:tq + dil * 127 + 1:dil]
                        e3 = ecount % 4
                        ecount += 1
                        pS = cx.psF[2 + e3][:, 0:256]
                        pskey = 'PS%d' % (2 + e3)
                        if has_prev:
                            tk = r + dil * 128 * (n - 1)
                            S.op('tensor', lambda e, tk=tk, qs=qs, pS=pS, dil=dil, p0=p0, p1=p1: e.matmul(pS[:, 0:128], lhsT=KT[p0:p1, tk:tk + dil * 127 + 1:dil], rhs=qs, start=True, stop=True),
                                 reads=['KT', 'QT'], writes=[pskey])
                        S.op('tensor', lambda e, tq=tq, qs=qs, pS=pS, dil=dil, p0=p0, p1=p1: e.matmul(pS[:, 128:256], lhsT=KT[p0:p1, tq:tq + dil * 127 + 1:dil], rhs=qs, start=True, stop=True),
                             reads=['KT', 'QT'], writes=[pskey])
                        S.op('scalar', lambda e, e3=e3, lo=lo, pS=pS: e.activation(out=Et[e3][:, lo:256], in_=pS[:, lo:256], func=AF.Exp), reads=[pskey], writes=['Et%d' % e3])
                        S.op('vector' if (ecount % 2 == 0) else 'gpsimd', lambda e, e3=e3, lo=lo, h=h, di=di: e.tensor_tensor(out=Pt[e3][:, lo:256], in0=Et[e3][:, lo:256], in1=wtab[:, h, di, lo:256], op=ALU.mult),
                             reads=['Et%d' % e3, 'wtab'], writes=['Pt%d' % e3])
                        pend[t] = (di, dil, r, n, e3, has_prev, tq)
                    if t - LAG >= 0:
                        di, dil, r, n, e3, has_prev, tq = pend.pop(t - LAG)
                        nbr = 32 // dil
                        o4 = ocount % 2
                        ocount += 1
                        pO = cx.psF[6 + o4][:, 0:128]
                        pokey = 'PS%d' % (6 + o4)
                        blk = r * nbr + n

                        def vaug(b_, di=di):
                            voff = di * 8192 + 256 * b_ + 128 * s
                            return Vt[:, voff:voff + 128]
                        if has_prev:
                            va = vaug(blk - 1)
                            S.op('tensor', lambda e, va=va, e3=e3, pO=pO: e.matmul(pO, lhsT=va, rhs=Pt[e3][:, 0:128], start=True, stop=False), reads=['Vt', 'Pt%d' % e3], writes=[pokey])
                        vb = vaug(blk)
                        S.op('tensor', lambda e, vb=vb, e3=e3, pO=pO, has_prev=has_prev: e.matmul(pO, lhsT=vb, rhs=Pt[e3][:, 128:256], start=(not has_prev), stop=True),
                             reads=['Vt', 'Pt%d' % e3], writes=[pokey])
                        tl = tq - 2048 * half
                        dstA = A[:, tl:tl + dil * 127 + 1:dil]
                        if di == 0:
                            S.op('scalar', lambda e, dstA=dstA, pO=pO: e.activation(out=dstA, in_=pO, func=AF.Copy), reads=[pokey], writes=[akey])
                        else:
                            S.op('vector', lambda e, dstA=dstA, pO=pO: e.tensor_tensor(out=dstA, in0=pO, in1=dstA, op=ALU.add), reads=[pokey, akey], writes=[akey])
                if s == 0:
                    nlo, dlo = 0, 64
                else:
                    nlo, dlo = 64, 0
                S.op('vector', lambda e, A=A, dlo=dlo: e.reciprocal(out=A[dlo:dlo + 64, :], in_=A[dlo:dlo + 64, :]), reads=[akey], writes=[akey])
                S.op('scalar', lambda e, A=A, dlo=dlo, nlo=nlo: e.activation(out=rd[nlo:nlo + 64, :], in_=A[dlo:dlo + 64, :], func=AF.Copy), reads=[akey], writes=['rd'])
                S.op('vector', lambda e, A=A, nlo=nlo, half=half: e.tensor_tensor(out=aT[half][nlo:nlo + 64, :], in0=A[nlo:nlo + 64, :], in1=rd[nlo:nlo + 64, :], op=ALU.mult),
                     reads=[akey, 'rd'], writes=['aT%d_%d' % (half, s)])
        for half in range(2):
            S.op('gpsimd', lambda e, hp=hp, half=half: e.dma_start(out=mixT_v[:, hp, 2048 * half:2048 * half + 2048], in_=aT[half]),
                 reads=['aT%d_0' % half, 'aT%d_1' % half], writes=['mixT'], dma_key='atst%d' % half)
    S.barrier(lambda e: e.memset(cx.scr, 0.0))
    M.release()


def phase_m4(S, cx, l):
    M = cx.mem
    M.mark()
    gt = M.alloc([D], F32)
    wo = M.alloc([8, D], BF16)
    stg = [M.alloc([2, D], F32) for _ in range(2)]
    mT = [M.alloc([8, 512], BF16) for _ in range(2)]
    xt = [M.alloc([D], F32) for _ in range(2)]
    xn = [M.alloc([D], F32) for _ in range(2)]
    load_mod_bc(S, cx, gt, l, 2, 'gt1')
    wsrc = cx.w_out[l].rearrange("(k p) n -> p k n", p=128)
    for q in range(4):
        S.op('sync', lambda e, q=q: e.dma_start(out=stg[q % 2], in_=wsrc[:, 2 * q:2 * q + 2, :]), writes=['wstg%d' % (q % 2)], dma_key='wstg%d' % (q % 2))
        S.op('gpsimd', lambda e, q=q: e.tensor_copy(out=wo[:, 2 * q:2 * q + 2, :], in_=stg[q % 2]), reads=['wstg%d' % (q % 2)], writes=['wo'])
    mixT_v = cx.mixT.rearrange("(k p) t -> p k t", p=128)
    xsrc = cx.x_h if l == 0 else cx.xres
    for tb in range(8):
        mb = tb % 2
        S.op('sync', lambda e, tb=tb, mb=mb: e.dma_start(out=mT[mb], in_=mixT_v[:, :, 512 * tb:512 * tb + 512]), reads=['mixT'], writes=['mT%d' % mb], dma_key='mT%d' % mb)
        for jj in range(4):
            j = 4 * tb + jj
            b = j % 2
            S.op('sync', lambda e, j=j, b=b: e.dma_start(out=xt[b], in_=xsrc[128 * j:128 * j + 128, :]), reads=['xres%d' % j], writes=['xt%d' % b], dma_key='xt%d' % b)
            for nh in range(2):
                po = cx.psF[2 * b + nh]
                for kc in range(8):
                    S.op('tensor', lambda e, kc=kc, jj=jj, nh=nh, mb=mb, po=po: e.matmul(po[:, :], lhsT=mT[mb][:, kc, 128 * jj:128 * jj + 128], rhs=wo[:, kc, 512 * nh:512 * nh + 512], start=(kc == 0), stop=(kc == 7)),
                         reads=['mT%d' % mb, 'wo'], writes=['PS%d' % (2 * b + nh)])
                S.op('vector', lambda e, nh=nh, b=b, po=po: e.tensor_tensor(out=xn[b][:, 512 * nh:512 * nh + 512], in0=po[:, :], in1=gt[:, 512 * nh:512 * nh + 512], op=ALU.mult),
                     reads=['PS%d' % (2 * b + nh), 'gt1'], writes=['xn%d' % b])
            S.op('gpsimd', lambda e, b=b: e.tensor_tensor(out=xn[b], in0=xn[b], in1=xt[b], op=ALU.add), reads=['xn%d' % b, 'xt%d' % b], writes=['xn%d' % b])
            S.op('scalar', lambda e, j=j, b=b: e.dma_start(out=cx.xres[128 * j:128 * j + 128, :], in_=xn[b]), reads=['xn%d' % b], writes=['xres%d' % j], dma_key='xst%d' % b)
    S.barrier(lambda e: e.memset(cx.scr, 0.0))
    M.release()


def phase_moe(S, cx, l, last):
    M = cx.mem
    st = cx.stack
    M.mark()
    desti = M.alloc([NT, 2], I32)
    wts = M.alloc([NT, 2], F32)
    ebi = M.alloc([64], I32)
    offi = M.alloc([64, 4], I32)
    M.mark()
    h2b = M.alloc([NT, D], BF16)
    g2 = M.alloc([D], F32)
    sc = M.alloc([D], F32)
    sh = M.alloc([D], F32)
    xt = [M.alloc([D], F32) for _ in range(2)]
    tmp = [M.alloc([D], F32) for _ in range(2)]
    h2f = [M.alloc([D], F32) for _ in range(2)]
    h2T = [M.alloc([8, 128], F32) for _ in range(2)]
    junk = M.alloc([D], BF16)
    ss = M.alloc([NT], F32)
    rs = M.alloc([NT], F32)
    wr = M.alloc([8, 36], F32)
    brb = M.alloc([36], F32)
    LG = M.alloc([NT, 36], F32)
    S.op('sync', lambda e: e.dma_start(out=g2, in_=_bc(cx.g_norm2[l:l + 1, :])), writes=['g2'], dma_key='e1g')
    load_mod_bc(S, cx, sh, l, 3, 'sh2')
    load_mod_bc(S, cx, sc, l, 4, 'sc2')
    S.op('vector', lambda e: e.scalar_tensor_tensor(out=g2, in0=sc, scalar=1.0, in1=g2, op0=ALU.add, op1=ALU.mult), reads=['sc2', 'g2'], writes=['g2'])
    S.op('sync', lambda e: e.dma_start(out=wr, in_=cx.w_r[l].rearrange("(p k) n -> p k n", k=8)), writes=['wr'], dma_key='e1w')
    S.op('sync', lambda e: e.dma_start(out=brb, in_=_bc(cx.b_r[l:l + 1, :])), writes=['brb'], dma_key='e1b')
    def e1_a(j):
        b = j % 2
        S.op('sync', lambda e, j=j, b=b: e.dma_start(out=xt[b], in_=cx.moe_x[128 * j:128 * j + 128, :]), reads=['xres%d' % j], writes=['xt%d' % b], dma_key='xt%d' % b)
        norm_mod_tile(S, cx, xt[b], 'xt%d' % b, g2, sh, None, None, j, ss, rs, junk)
        S.op('vector', lambda e, j=j, b=b: e.scalar_tensor_tensor(out=tmp[b], in0=xt[b], scalar=rs[:, j:j + 1], in1=g2, op0=ALU.mult, op1=ALU.mult),
             reads=['xt%d' % b, 'rs%d' % j, 'g2'], writes=['tmp%d' % b])
        S.op('gpsimd', lambda e, b=b: e.tensor_tensor(out=h2f[b], in0=tmp[b], in1=sh, op=ALU.add), reads=['tmp%d' % b, 'sh2'], writes=['h2f%d' % b])
        S.op('gpsimd', lambda e, j=j, b=b: e.tensor_copy(out=h2b[:, j, :], in_=h2f[b]), reads=['h2f%d' % b], writes=['h2b%d' % j])

    def e1_b(j):
        b = j % 2
        for half in range(2):
            pT = cx.psF[2 * b + half]
            for k4 in range(4):
                k = 4 * half + k4
                S.op('tensor', lambda e, k=k, k4=k4, b=b, pT=pT: e.transpose(pT[:, 128 * k4:128 * k4 + 128], h2f[b][:, k:D:8], cx.identF), reads=['h2f%d' % b, 'identF'], writes=['PS%d' % (2 * b + half)])
            S.op('scalar' if half == 0 else 'vector',
                 (lambda e, b=b, half=half, pT=pT: e.activation(out=h2T[b][:, 4 * half:4 * half + 4, :], in_=pT.rearrange("p (k t) -> p k t", k=4), func=AF.Copy)) if half == 0 else
                 (lambda e, b=b, half=half, pT=pT: e.tensor_copy(out=h2T[b][:, 4 * half:4 * half + 4, :], in_=pT.rearrange("p (k t) -> p k t", k=4))),
                 reads=['PS%d' % (2 * b + half)], writes=['h2T%d_%d' % (b, half)])
        pl = cx.psF[4 + b]
        for k in range(8):
            S.op('tensor', lambda e, k=k, b=b, pl=pl: e.matmul(pl[:, 0:36], lhsT=h2T[b][:, k, :], rhs=wr[:, k, :], start=(k == 0), stop=(k == 7)),
                 reads=['h2T%d_%d' % (b, k // 4), 'wr'], writes=['PS%d' % (4 + b)])
        S.op('vector', lambda e, j=j, pl=pl: e.tensor_tensor(out=LG[:, j, :], in0=pl[:, 0:36], in1=brb, op=ALU.add), reads=['PS%d' % (4 + b), 'brb'], writes=['LG'])
    e1_a(0)
    for j in range(NT):
        if j + 1 < NT:
            e1_a(j + 1)
        e1_b(j)
    if cx.dbg:
        S.op('sync', lambda e: e.dma_start(out=cx.dbg_h2.ap().rearrange("(j p) d -> p j d", p=128), in_=h2b), reads=['h2b%d' % j for j in range(NT)], writes=['dbg_h2'], dma_key='dbg5')
        S.op('sync', lambda e: e.dma_start(out=cx.dbg_rs.ap(), in_=rs), reads=['rs%d' % j for j in range(NT)], writes=['dbg_rs'], dma_key='dbg6')
    gmx = M.alloc([NT], F32)
    gsel = M.alloc([NT, 4], F32)
    gex = M.alloc([NT, 4], F32)
    pg = M.alloc([NT], F32)
    EM = M.alloc([NT, 32], F32)
    m1 = M.alloc([NT], F32)
    m2 = M.alloc([NT], F32)
    sel1 = M.alloc([NT, 32], F32)
    sel2 = M.alloc([NT, 32], F32)
    Ab = M.alloc([NT, 32], BF16)
    Rk = M.alloc([NT, 32], F32)
    Tot = M.alloc([NT, 32], F32)
    TP = M.alloc([NT, 32], F32)
    cnt = M.alloc([32], F32)
    TH = M.alloc([32, 16], F32)
    BI = M.alloc([NB, 32], F32)
    cmp1 = M.alloc([32, 16], F32)
    cmp2 = M.alloc([NB, 32], F32)
    nblk = M.alloc([32], F32)
    bend = M.alloc([32], F32)
    base = M.alloc([32], F32)
    one32 = M.alloc([32], F32)
    ebf = M.alloc([64], F32)
    destf = M.alloc([NT, 2], F32)
    t1 = M.alloc([NT], F32)
    S.op('sync', lambda e: e.dma_start(out=TH, in_=cx.cf32[:, 256:256 + 512].rearrange("p (a b) -> p a b", a=32)), writes=['TH'], dma_key='e2a')
    S.op('sync', lambda e: e.dma_start(out=BI, in_=cx.cf32[:, 768:768 + NB * 32].rearrange("p (a b) -> p a b", a=NB)), writes=['BI'], dma_key='e2b')
    V = 'vector'
    G = LG[:, :, 0:4]
    EL = LG[:, :, 4:36]
    S.op(V, lambda e: e.tensor_reduce(out=gmx, in_=G, axis=AX.X, op=ALU.max), reads=['LG'], writes=['gmx'])
    S.op(V, lambda e: e.tensor_tensor(out=gsel, in0=G, in1=gmx.unsqueeze(2).to_broadcast([128, NT, 4]), op=ALU.is_equal), reads=['LG', 'gmx'], writes=['gsel'])
    S.op(V, lambda e: e.tensor_tensor(out=gex, in0=G, in1=gmx.unsqueeze(2).to_broadcast([128, NT, 4]), op=ALU.subtract), reads=['LG', 'gmx'], writes=['gex'])
    S.op('scalar', lambda e: e.activation(out=gex, in_=gex, func=AF.Exp), reads=['gex'], writes=['gex'])
    S.op(V, lambda e: e.tensor_reduce(out=pg, in_=gex, axis=AX.X, op=ALU.add), reads=['gex'], writes=['pg'])
    S.op(V, lambda e: e.reciprocal(out=pg, in_=pg), reads=['pg'], writes=['pg'])
    S.op(V, lambda e: e.tensor_scalar(out=gex, in0=gsel, scalar1=BIG, scalar2=-BIG, op0=ALU.mult, op1=ALU.add), reads=['gsel', 'gex'], writes=['gex'])
    S.op(V, lambda e: e.tensor_tensor(out=EM.rearrange("p j (g x) -> p j g x", g=4), in0=EL.rearrange("p j (g x) -> p j g x", g=4),
                                      in1=gex.unsqueeze(3).to_broadcast([128, NT, 4, 8]), op=ALU.add), reads=['LG', 'gex'], writes=['EM'])
    S.op(V, lambda e: e.tensor_reduce(out=m1, in_=EM, axis=AX.X, op=ALU.max), reads=['EM'], writes=['m1'])
    S.op(V, lambda e: e.tensor_tensor(out=sel1, in0=EM, in1=m1.unsqueeze(2).to_broadcast([128, NT, 32]), op=ALU.is_equal), reads=['EM', 'm1'], writes=['sel1'])
    S.op(V, lambda e: e.scalar_tensor_tensor(out=EM, in0=sel1, scalar=-BIG, in1=EM, op0=ALU.mult, op1=ALU.add), reads=['sel1', 'EM'], writes=['EM'])
    S.op(V, lambda e: e.tensor_reduce(out=m2, in_=EM, axis=AX.X, op=ALU.max), reads=['EM'], writes=['m2'])
    S.op(V, lambda e: e.tensor_tensor(out=sel2, in0=EM, in1=m2.unsqueeze(2).to_broadcast([128, NT, 32]), op=ALU.is_equal), reads=['EM', 'm2'], writes=['sel2'])
    S.op(V, lambda e: e.tensor_tensor(out=t1, in0=m2, in1=m1, op=ALU.subtract), reads=['m1', 'm2'], writes=['t1'])
    S.op('scalar', lambda e: e.activation(out=t1, in_=t1, func=AF.Exp), reads=['t1'], writes=['t1'])
    S.op(V, lambda e: e.tensor_scalar(out=t1, in0=t1, scalar1=1.0, scalar2=None, op0=ALU.add), reads=['t1'], writes=['t1'])
    S.op(V, lambda e: e.reciprocal(out=t1, in_=t1), reads=['t1'], writes=['t1'])
    S.op(V, lambda e: e.tensor_tensor(out=wts[:, :, 0], in0=t1, in1=pg, op=ALU.mult), reads=['t1', 'pg'], writes=['wts'])
    S.op(V, lambda e: e.tensor_tensor(out=wts[:, :, 1], in0=pg, in1=wts[:, :, 0], op=ALU.subtract), reads=['pg', 'wts'], writes=['wts'])
    S.op(V, lambda e: e.tensor_tensor(out=Ab, in0=sel1, in1=sel2, op=ALU.add), reads=['sel1', 'sel2'], writes=['Ab'])
    Abf = Ab.rearrange("p j x -> p (j x)")
    for hh in range(2):
        pr = cx.psF[hh]
        pt = cx.psF[2 + hh]
        S.op('tensor', lambda e, hh=hh, pr=pr: e.matmul(pr[:, :], lhsT=cx.rstrictB, rhs=Abf[:, 512 * hh:512 * hh + 512], start=True, stop=True), reads=['rstrictB', 'Ab'], writes=['PS%d' % hh])
        S.op('tensor', lambda e, hh=hh, pt=pt: e.matmul(pt[:, :], lhsT=cx.onesB, rhs=Abf[:, 512 * hh:512 * hh + 512], start=True, stop=True), reads=['onesB', 'Ab'], writes=['PS%d' % (2 + hh)])
        S.op('scalar', lambda e, hh=hh, pr=pr: e.activation(out=Rk.rearrange("p j x -> p (j x)")[:, 512 * hh:512 * hh + 512], in_=pr[:, :], func=AF.Copy), reads=['PS%d' % hh], writes=['Rk'])
        S.op(V, lambda e, hh=hh, pt=pt: e.tensor_copy(out=Tot.rearrange("p j x -> p (j x)")[:, 512 * hh:512 * hh + 512], in_=pt[:, :]), reads=['PS%d' % (2 + hh)], writes=['Tot'])
    S.op(V, lambda e: e.memset(TP[:, 0, :], 0.0), writes=['TP'])
    for j in range(1, NT):
        S.op(V, lambda e, j=j: e.tensor_tensor(out=TP[:, j, :], in0=TP[:, j - 1, :], in1=Tot[:, j - 1, :], op=ALU.add), reads=['TP', 'Tot'], writes=['TP'])
    S.op(V, lambda e: e.tensor_tensor(out=cnt, in0=TP[:, NT - 1, :], in1=Tot[:, NT - 1, :], op=ALU.add), reads=['TP', 'Tot'], writes=['cnt'])
    S.op(V, lambda e: e.tensor_tensor(out=cmp1, in0=cnt.unsqueeze(2).to_broadcast([128, 32, 16]), in1=TH, op=ALU.is_gt), reads=['cnt', 'TH'], writes=['cmp1'])
    S.op(V, lambda e: e.tensor_reduce(out=nblk, in_=cmp1, axis=AX.X, op=ALU.add), reads=['cmp1'], writes=['nblk'])
    S.op(V, lambda e: e.memset(one32, 1.0), writes=['one32'])
    S.op(V, lambda e: e.tensor_tensor_scan(out=bend, data0=one32, data1=nblk, initial=0.0, op0=ALU.mult, op1=ALU.add), reads=['one32', 'nblk'], writes=['bend'])
    S.op(V, lambda e: e.tensor_tensor(out=base, in0=bend, in1=nblk, op=ALU.subtract), reads=['bend', 'nblk'], writes=['base'])
    S.op(V, lambda e: e.tensor_scalar(out=base, in0=base, scalar1=float(BS), scalar2=None, op0=ALU.mult), reads=['base'], writes=['base'])
    S.op(V, lambda e: e.tensor_tensor(out=Rk, in0=Rk, in1=TP, op=ALU.add), reads=['Rk', 'TP'], writes=['Rk'])
    S.op(V, lambda e: e.tensor_tensor(out=Rk, in0=Rk, in1=base.unsqueeze(1).to_broadcast([128, NT, 32]), op=ALU.add), reads=['Rk', 'base'], writes=['Rk'])
    S.op(V, lambda e: e.tensor_tensor(out=sel1, in0=sel1, in1=Rk, op=ALU.mult), reads=['sel1', 'Rk'], writes=['sel1'])
    S.op(V, lambda e: e.tensor_tensor(out=sel2, in0=sel2, in1=Rk, op=ALU.mult), reads=['sel2', 'Rk'], writes=['sel2'])
    S.op(V, lambda e: e.tensor_reduce(out=destf[:, :, 0], in_=sel1, axis=AX.X, op=ALU.add), reads=['sel1'], writes=['destf'])
    S.op(V, lambda e: e.tensor_reduce(out=destf[:, :, 1], in_=sel2, axis=AX.X, op=ALU.add), reads=['sel2'], writes=['destf'])
    S.op(V, lambda e: e.tensor_copy(out=desti, in_=destf), reads=['destf'], writes=['desti'])
    S.op(V, lambda e: e.tensor_tensor(out=cmp2, in0=bend.unsqueeze(1).to_broadcast([128, NB, 32]), in1=BI, op=ALU.is_le), reads=['bend', 'BI'], writes=['cmp2'])
    S.op(V, lambda e: e.memset(ebf, 31.0), writes=['ebf'])
    S.op(V, lambda e: e.tensor_reduce(out=ebf[:, 0:NB], in_=cmp2, axis=AX.X, op=ALU.add), reads=['cmp2'], writes=['ebf'])
    einv = M.alloc([64], F32)
    S.op(V, lambda e: e.tensor_scalar(out=einv, in0=ebf, scalar1=31.5, scalar2=268435456.0, op0=ALU.is_gt, op1=ALU.mult), reads=['ebf'], writes=['einv'])
    S.op(V, lambda e: e.tensor_scalar(out=ebf, in0=ebf, scalar1=31.0, scalar2=None, op0=ALU.min), reads=['ebf'], writes=['ebf'])
    S.op(V, lambda e: e.tensor_copy(out=ebi, in_=ebf), reads=['ebf'], writes=['ebi'])
    offf = M.alloc([64, 4], F32)
    for q in range(4):
        S.op(V, lambda e, q=q: e.tensor_scalar(out=offf[:, :, q], in0=ebf, scalar1=2097152.0, scalar2=float(l * 32 * 2097152 + ((q % 2) * 8192 if q < 2 else (q % 2) * 1048576)), op0=ALU.mult, op1=ALU.add), reads=['ebf'], writes=['offf'])
    S.op(V, lambda e: e.tensor_tensor(out=offf, in0=offf, in1=einv.unsqueeze(2).to_broadcast([128, 64, 4]), op=ALU.add), reads=['offf', 'einv'], writes=['offf'])
    S.op(V, lambda e: e.tensor_copy(out=offi, in_=offf), reads=['offf'], writes=['offi'])
    if cx.dbg:
        S.op('sync', lambda e: e.dma_start(out=cx.dbg_lg.ap().rearrange("(j p) n -> p j n", p=128), in_=LG), reads=['LG'], writes=['dbg_lg'], dma_key='dbg1')
        S.op('sync', lambda e: e.dma_start(out=cx.dbg_dest.ap().rearrange("(j p) n -> p j n", p=128), in_=desti), reads=['desti'], writes=['dbg_dest'], dma_key='dbg2')
        S.op('sync', lambda e: e.dma_start(out=cx.dbg_wts.ap().rearrange("(j p) n -> p j n", p=128), in_=wts), reads=['wts'], writes=['dbg_wts'], dma_key='dbg3')
        S.op('sync', lambda e: e.dma_start(out=cx.dbg_eb.ap(), in_=ebi[0:1, :]), reads=['ebi'], writes=['dbg_eb'], dma_key='dbg4')
    if getattr(cx, 'moe_stop', None) == 'e2':
        S.barrier(lambda e: e.memset(cx.scr, 0.0))
        M.release()
        M.release()
        return
    for j in range(NT):
        for k in range(2):
            S.op('gpsimd', lambda e, j=j, k=k: e.indirect_dma_start(out=cx.xslots[:, :], out_offset=bass.IndirectOffsetOnAxis(ap=desti[:, j, k:k + 1], axis=0),
                                                                    in_=h2b[:, j, :], in_offset=None, bounds_check=_bnd(cx, e), oob_is_err=False),
                 reads=['desti', 'h2b%d' % j], writes=['xslots'], dma_key='disp%d' % ((2 * j + k) % 4))
    S.barrier(lambda e: e.memset(cx.scr, 0.0))
    M.release()
    if getattr(cx, 'moe_stop', None) == 'disp':
        M.release()
        return
    M.mark()
    NSTG = 10
    stgb = [M.alloc([8192], U8) for _ in range(NSTG)]
    stg = [a.bitcast(F32).rearrange("p (a b) -> p a b", a=4) for a in stgb]
    wg = [M.alloc([8, 512], BF16) for _ in range(3)]
    wu = [M.alloc([8, 512], BF16) for _ in range(3)]
    wd = [M.alloc([4, D], BF16) for _ in range(4)]
    Xb = [M.alloc([2, D], BF16) for _ in range(2)]
    XT = [M.alloc([8, BS], BF16) for _ in range(2)]
    AT = [M.alloc([4, BS], BF16) for _ in range(2)]
    sg = [M.alloc([BS], F32) for _ in range(2)]
    Ysb = [M.alloc([2, D], F32) for _ in range(2)]
    if 'ereg' not in cx.regs:
        cx.regs['ereg'] = None
    cnt3 = {'sidx': 0, 'g': 0, 'y': 0}
    NBL = getattr(cx, 'nb_limit', NB)

    def loadw(e, b, piece, sl):
        wt_h = (cx.w_gate, cx.w_up, cx.w_down)[piece // 2]
        wbt = wt_h.ap().bitcast(U8).tensor
        if cx.regs['ereg'] is None:
            cx.regs['ereg'] = st.enter_context(e.register("dynoff"))
        r = cx.regs['ereg']
        oc = (piece % 2) + (2 if piece >= 4 else 0)
        e.reg_load(r, offi[0:1, b, oc:oc + 1])
        v = e.snap(r, min_val=0, max_val=268435456 + 127 * 2097152 + 1048576)
        if piece < 4:
            src = bass.AP(wbt, v, [[16384, 128], [1, 8192]])
            ins = e.dma_start(out=stgb[sl], in_=src, bounds_check='skip_entire_dma')
        else:
            src = bass.AP(wbt, v, [[4096, 128], [128 * 4096, 2], [1, 4096]])
            ins = e.dma_start(out=stgb[sl].rearrange("p (c n) -> p c n", c=2), in_=src, bounds_check='skip_entire_dma')
        e.free_register(v.val)
        return ins

    def st_w(b):
        wbuf = b % 3
        w3 = b % 4
        for piece in range(6):
            sl = cnt3['sidx'] % NSTG
            cnt3['sidx'] += 1
            S.op('sync', lambda e, b=b, piece=piece, sl=sl: loadw(e, b, piece, sl), reads=['offi'], writes=['stg%d' % sl], dma_key='stg%d' % sl)
            if piece < 4:
                dstw = (wg if piece < 2 else wu)[wbuf]
                kk = 4 * (piece % 2)
                wkey = ('wg%d' if piece < 2 else 'wu%d') % wbuf
                if piece < 2:
                    S.op('vector', lambda e, dstw=dstw, kk=kk, sl=sl: e.tensor_copy(out=dstw[:, kk:kk + 4, :], in_=stg[sl]), reads=['stg%d' % sl], writes=[wkey])
                else:
                    S.op('scalar', lambda e, dstw=dstw, kk=kk, sl=sl: e.activation(out=dstw[:, kk:kk + 4, :], in_=stg[sl], func=AF.Copy), reads=['stg%d' % sl], writes=[wkey])
            else:
                cc = 2 * (piece - 4)
                srcv = stg[sl].rearrange("p a b -> p (a b)").rearrange("p (c n) -> p c n", c=2)
                if piece == 4:
                    S.op('vector', lambda e, cc=cc, srcv=srcv, w3=w3: e.tensor_copy(out=wd[w3][:, cc:cc + 2, :], in_=srcv), reads=['stg%d' % sl], writes=['wd%d' % w3])
                else:
                    S.op('scalar', lambda e, cc=cc, srcv=srcv, w3=w3: e.activation(out=wd[w3][:, cc:cc + 2, :], in_=srcv, func=AF.Copy), reads=['stg%d' % sl], writes=['wd%d' % w3])

    def st_t(b):
        xb = b % 2
        S.op('sync', lambda e, b=b, xb=xb: e.dma_start(out=Xb[xb], in_=cx.xslots[BS * b:BS * b + BS, :].rearrange("(s p) d -> p s d", p=128)), reads=['xslots'], writes=['Xb%d' % xb], dma_key='Xb%d' % xb)
        for sblk in range(2):
            pX = cx.psB[sblk]
            for k in range(8):
                S.op('tensor', lambda e, k=k, sblk=sblk, xb=xb, pX=pX: e.transpose(pX[:, 128 * k:128 * k + 128], Xb[xb][:, sblk, k:D:8], cx.identB), reads=['Xb%d' % xb, 'identB'], writes=['PS%d' % sblk])
            if sblk == 0:
                S.op('scalar', lambda e, xb=xb, pX=pX: e.activation(out=XT[xb][:, :, 0:128], in_=pX.rearrange("p (k t) -> p k t", k=8), func=AF.Copy), reads=['PS0'], writes=['XT%d' % xb])
            else:
                S.op('vector', lambda e, xb=xb, pX=pX: e.tensor_copy(out=XT[xb][:, :, 128:256], in_=pX.rearrange("p (k t) -> p k t", k=8)), reads=['PS1'], writes=['XT%d' % xb])

    def st_gu(b):
        xb = b % 2
        wbuf = b % 3
        for c in range(4):
            g2_ = cnt3['g'] % 2
            cnt3['g'] += 1
            pG = cx.psF[2 + g2_][:, 0:BS]
            pU = cx.psF[4 + g2_][:, 0:BS]
            for k in range(8):
                S.op('tensor', lambda e, k=k, c=c, xb=xb, wbuf=wbuf, pG=pG: e.matmul(pG, lhsT=wg[wbuf][:, k, 128 * c:128 * c + 128], rhs=XT[xb][:, k, :], start=(k == 0), stop=(k == 7)),
                     reads=['wg%d' % wbuf, 'XT%d' % xb], writes=['PS%d' % (2 + g2_)])
            S.op('scalar', lambda e, g2_=g2_, pG=pG: e.activation(out=sg[g2_], in_=pG, func=AF.Silu), reads=['PS%d' % (2 + g2_)], writes=['sg%d' % g2_])
            for k in range(8):
                S.op('tensor', lambda e, k=k, c=c, xb=xb, wbuf=wbuf, pU=pU: e.matmul(pU, lhsT=wu[wbuf][:, k, 128 * c:128 * c + 128], rhs=XT[xb][:, k, :], start=(k == 0), stop=(k == 7)),
                     reads=['wu%d' % wbuf, 'XT%d' % xb], writes=['PS%d' % (4 + g2_)])
            S.op('vector', lambda e, c=c, xb=xb, g2_=g2_, pU=pU: e.tensor_tensor(out=AT[xb][:, c, :], in0=pU, in1=sg[g2_], op=ALU.mult), reads=['PS%d' % (4 + g2_), 'sg%d' % g2_], writes=['AT%d' % xb])

    def st_y(b):
        xb = b % 2
        w3 = b % 4
        for sblk in range(2):
            for nh in range(2):
                pY = cx.psF[6 + (cnt3['y'] % 2)]
                pk = 'PS%d' % (6 + (cnt3['y'] % 2))
                cnt3['y'] += 1
                for c in range(4):
                    S.op('tensor', lambda e, c=c, sblk=sblk, nh=nh, xb=xb, w3=w3, pY=pY: e.matmul(pY[:, :], lhsT=AT[xb][:, c, 128 * sblk:128 * sblk + 128], rhs=wd[w3][:, c, 512 * nh:512 * nh + 512], start=(c == 0), stop=(c == 3)),
                         reads=['AT%d' % xb, 'wd%d' % w3], writes=[pk])
                if nh == 0:
                    S.op('scalar', lambda e, sblk=sblk, xb=xb, pY=pY: e.activation(out=Ysb[xb][:, sblk, 0:512], in_=pY[:, :], func=AF.Copy), reads=[pk], writes=['Ysb%d' % xb])
                else:
                    S.op('vector', lambda e, sblk=sblk, xb=xb, pY=pY: e.tensor_copy(out=Ysb[xb][:, sblk, 512:1024], in_=pY[:, :]), reads=[pk], writes=['Ysb%d' % xb])
        S.op('gpsimd', lambda e, b=b, xb=xb: e.dma_start(out=cx.yslots[BS * b:BS * b + BS, :].rearrange("(s p) d -> p s d", p=128), in_=Ysb[xb]), reads=['Ysb%d' % xb], writes=['yslots'], dma_key='yst%d' % xb)

    st_w(0)
    if NBL > 1:
        st_w(1)
    for it in range(NBL + 2):
        if it < NBL:
            st_t(it)
        if 0 <= it - 1 < NBL:
            st_gu(it - 1)
        if 0 <= it - 2 < NBL:
            st_y(it - 2)
        if it + 2 < NBL:
            st_w(it + 2)
    S.barrier(lambda e: e.memset(cx.scr, 0.0))
    M.release()
    if getattr(cx, 'moe_stop', None) == 'e3':
        M.release()
        return
    M.mark()
    gt4 = M.alloc([D], F32)
    Y1 = [M.alloc([D], F32) for _ in range(3)]
    Y2 = [M.alloc([D], F32) for _ in range(3)]
    xt4 = [M.alloc([D], F32) for _ in range(3)]
    xo4 = [M.alloc([D], F32) for _ in range(2)]
    yo4 = [M.alloc([D], F32) for _ in range(2)]
    load_mod_bc(S, cx, gt4, l, 5, 'gt2')
    if last:
        gf4 = M.alloc([D], F32)
        ss4 = M.alloc([NT], F32)
        rs4 = M.alloc([NT], F32)
        junk4 = M.alloc([D], BF16)
        S.op('sync', lambda e: e.dma_start(out=gf4, in_=_bc(cx.g_final.ap())), writes=['gf'], dma_key='gf')
    def e4_a(j):
        b = j % 3
        S.op('sync', lambda e, j=j, b=b: e.dma_start(out=xt4[b], in_=cx.moe_x[128 * j:128 * j + 128, :]), reads=['xres%d' % j], writes=['xt%d' % b], dma_key='xt%d' % b)
        S.op('gpsimd', lambda e, j=j, b=b: e.indirect_dma_start(out=Y1[b], out_offset=None, in_=cx.yslots[:, :], in_offset=bass.IndirectOffsetOnAxis(ap=desti[:, j, 0:1], axis=0),
                                                                bounds_check=_bnd(cx, e), oob_is_err=False), reads=['desti', 'yslots'], writes=['Y1_%d' % b], dma_key='Y1_%d' % b)
        S.op('gpsimd', lambda e, j=j, b=b: e.indirect_dma_start(out=Y2[b], out_offset=None, in_=cx.yslots[:, :], in_offset=bass.IndirectOffsetOnAxis(ap=desti[:, j, 1:2], axis=0),
                                                                bounds_check=_bnd(cx, e), oob_is_err=False), reads=['desti', 'yslots'], writes=['Y2_%d' % b], dma_key='Y2_%d' % b)

    def e4_b(j):
        b = j % 2
        g = j % 3
        S.op('vector', lambda e, j=j, b=b, g=g: e.tensor_scalar(out=Y1[g], in0=Y1[g], scalar1=wts[:, j, 0:1], scalar2=None, op0=ALU.mult), reads=['Y1_%d' % g, 'wts'], writes=['Y1_%d' % g])
        S.op('vector', lambda e, j=j, b=b, g=g: e.scalar_tensor_tensor(out=Y1[g], in0=Y2[g], scalar=wts[:, j, 1:2], in1=Y1[g], op0=ALU.mult, op1=ALU.add), reads=['Y1_%d' % g, 'Y2_%d' % g, 'wts'], writes=['Y1_%d' % g])
        S.op('gpsimd', lambda e, b=b, g=g: e.tensor_tensor(out=Y1[g], in0=Y1[g], in1=gt4, op=ALU.mult), reads=['Y1_%d' % g, 'gt2'], writes=['Y1_%d' % g])
        S.op('vector', lambda e, b=b, g=g: e.tensor_tensor(out=xo4[b], in0=xt4[g], in1=Y1[g], op=ALU.add), reads=['Y1_%d' % g, 'xt%d' % g], writes=['xo%d' % b])
        if not last:
            S.op('scalar', lambda e, j=j, b=b, g=g: e.dma_start(out=cx.xres[128 * j:128 * j + 128, :], in_=xo4[b]), reads=['xo%d' % b], writes=['xres%d' % j], dma_key='xst%d' % b)
        else:
            norm_mod_tile(S, cx, xo4[b], 'xo%d' % b, None, None, None, None, j, ss4, rs4, junk4)
            S.op('vector', lambda e, j=j, b=b, g=g: e.scalar_tensor_tensor(out=yo4[b], in0=xo4[b], scalar=rs4[:, j:j + 1], in1=gf4, op0=ALU.mult, op1=ALU.mult), reads=['xo%d' % b, 'rs%d' % j, 'gf'], writes=['yo%d' % b])
            o = S.op('scalar', lambda e, j=j, b=b, g=g: e.dma_start(out=cx.out_h[128 * j:128 * j + 128, :], in_=yo4[b]), reads=['yo%d' % b], writes=['out%d' % j], dma_key='ost%d' % b)
            S.final(o)
    e4_a(0)
    e4_a(1)
    for j in range(NT):
        if j + 2 < NT:
            e4_a(j + 2)
        e4_b(j)
    S.barrier(lambda e: e.memset(cx.scr, 0.0))
    M.release()
    M.release()


def build_program(n_layers=DEPTH, dbg=False, phases=None, moe_stop=None, nb_limit=NB, static_w=False):
    nc = bass.Bass("TRN2", target_bir_lowering=False)
    cx = Ctx()
    cx.n_layers = n_layers
    cx.dbg = dbg
    cx.regs = {}
    cx.moe_stop = moe_stop
    cx.nb_limit = nb_limit
    cx.static_w = static_w
    L = DEPTH

    def din(name, shape, d=F32):
        return nc.dram_tensor(name, shape, d, kind="ExternalInput")
    cx.x_h = din("x", [S_LEN, D])
    cx.c_h = din("c", [1, D])
    cx.w_mod = din("w_mod", [L, D, 6 * D])
    cx.b_mod = din("b_mod", [L, 6 * D])
    cx.g_norm1 = din("g_norm1", [L, D])
    cx.w_in = din("w_in", [L, D, DIN])
    cx.conv_pack = din("conv_pack", [L, 34, 512])
    cx.w_out = din("w_out", [L, D, D])
    cx.g_norm2 = din("g_norm2", [L, D])
    cx.w_r = din("w_r", [L, D, 36])
    cx.b_r = din("b_r", [L, 36])
    cx.w_gate = din("w_exp_gate", [L, NE, D, DE])
    cx.w_up = din("w_exp_up", [L, NE, D, DE])
    cx.w_down = din("w_exp_down", [L, NE, DE, D])
    cx.g_final = din("g_final", [1, D])
    cx.cf32 = din("cf32", [128, 768 + NB * 32])
    cx.cbf = din("cbf", [128, 384], BF16)
    cx.wtab_h = din("wtab", [128, NH * 3 * 256], BF16)
    cx.out_h = nc.dram_tensor("out", [S_LEN, D], F32, kind="ExternalOutput")
    skind = "ExternalOutput" if dbg else "Internal"
    cx.xres = nc.dram_tensor("xres", [S_LEN, D], F32, kind=skind)
    cx.mixT = nc.dram_tensor("mixT", [D, S_LEN], BF16, kind=skind)
    cx.modscr = nc.dram_tensor("modscr", [L, 6 * D], F32, kind=skind)
    cx.xslots = nc.dram_tensor("xslots", [NSLOT, D], BF16, kind="Internal")
    cx.yslots = nc.dram_tensor("yslots", [NSLOT, D], F32, kind="Internal")
    if dbg:
        cx.dbg_lg = nc.dram_tensor("dbg_lg", [S_LEN, 36], F32, kind="ExternalOutput")
        cx.dbg_dest = nc.dram_tensor("dbg_dest", [S_LEN, 2], I32, kind="ExternalOutput")
        cx.dbg_wts = nc.dram_tensor("dbg_wts", [S_LEN, 2], F32, kind="ExternalOutput")
        cx.dbg_eb = nc.dram_tensor("dbg_eb", [1, 64], I32, kind="ExternalOutput")
        cx.dbg_h2 = nc.dram_tensor("dbg_h2", [S_LEN, D], BF16, kind="ExternalOutput")
        cx.dbg_rs = nc.dram_tensor("dbg_rs", [128, NT], F32, kind="ExternalOutput")
        cx.dbg_hT = nc.dram_tensor("dbg_hT", [128, 8 * S_LEN], BF16, kind="ExternalOutput")
    with ExitStack() as st:
        cx.stack = st
        pool = st.enter_context(nc.sbuf_tensor("pool", [128, POOLB], dt.uint8))
        cx.mem = Mem(pool, POOLB)
        cx.psF = [st.enter_context(nc.psum_tensor("psb%d" % i, [128, 512], F32)) for i in range(8)]
        cx.psB = [p[:, :].bitcast(BF16) for p in cx.psF]
        cx.psF = [p[:, :] for p in cx.psF]
        S = Sched(nc)
        cx.epsT = cx.mem.alloc([1], F32)
        S.op('vector', lambda e: e.memset(cx.epsT, EPS), writes=['epsT'])
        phase_prologue(S, cx)
        cx.hT = None
        for l in range(n_layers):
            last = (l == n_layers - 1)
            cx.mem.mark()
            cx.hT = cx.mem.alloc([8, S_LEN], BF16)
            if phases is None or 'm1' in phases:
                phase_m1(S, cx, l)
                if dbg and l == 0:
                    S.op('sync', lambda e: e.dma_start(out=cx.dbg_hT.ap(), in_=cx.hT.rearrange("p k t -> p (k t)")), reads=['hT'], writes=['dbg_hT'], dma_key='dbg0')
            if phases is None or 'm2' in phases:
                phase_m2(S, cx, l)
            if phases is None or 'm3' in phases:
                phase_m3(S, cx, l)
            if phases is None or 'm4' in phases:
                phase_m4(S, cx, l)
            cx.mem.release()
            cx.moe_x = cx.xres if (phases is None or 'm4' in phases or l > 0) else cx.x_h
            if phases is None or 'moe' in phases:
                phase_moe(S, cx, l, last)
        for k, o in list(S.last_w.items()):
            if isinstance(k, str) and k.startswith('dbg'):
                S.final(o)
        endop = S.barrier(lambda e: e.memset(cx.scr, 0.0))
        S.final(endop)
        S.emit(st)
        cx.n_ops = S.nops
        cx.peak = cx.mem.peak
    return nc, cx


def host_consts():
    cf = np.zeros((128, 768 + NB * 32), np.float32)
    cf[:, 0:128] = np.eye(128, dtype=np.float32)
    cf[:, 128:256] = 1.0 / 512.0
    th = (np.arange(16, dtype=np.float32) * BS)[None, :].repeat(32, 0)
    cf[:, 256:768] = th.reshape(-1)[None, :]
    bi = np.arange(NB, dtype=np.float32)[:, None].repeat(32, 1)
    cf[:, 768:] = bi.reshape(-1)[None, :]
    cb = np.zeros((128, 384), np.float32)
    cb[:, 0:128] = np.eye(128)
    cb[:, 128:256] = 1.0
    pp = np.arange(128)
    cb[:, 256:384] = (pp[:, None] < pp[None, :]).astype(np.float32)
    cb = cb.astype(ml_dtypes.bfloat16)
    slopes = 2.0 ** (-8.0 * np.arange(1, NH + 1) / NH)
    wt = np.zeros((128, NH, 3, 256), np.float64)
    ki = np.arange(128)[:, None]
    for bi_, dil in enumerate(DILS):
        for h in range(NH):
            qi = np.arange(128)[None, :]
            d_prev = 128 + qi - ki
            v_prev = (d_prev >= 0) & (d_prev <= 128)
            d_cur = qi - ki
            v_cur = (d_cur >= 0) & (d_cur <= 128)
            wt[:, h, bi_, 0:128] = np.where(v_prev, np.exp(-slopes[h] * dil * np.where(v_prev, d_prev, 0)), 0.0)
            wt[:, h, bi_, 128:256] = np.where(v_cur, np.exp(-slopes[h] * dil * np.where(v_cur, d_cur, 0)), 0.0)
    return cf, cb, wt.reshape(128, -1).astype(np.float32).astype(ml_dtypes.bfloat16)


_CACHE = {}


def make_in_maps(inputs, n_cores=8):
    f = lambda a: np.ascontiguousarray(np.asarray(a, dtype=np.float32))
    cf, cb, wt = host_consts()
    conv_pack = np.concatenate([f(inputs['conv_w']), f(inputs['conv_b'])[:, None, :], f(inputs['conv_ln_g'])[:, None, :], f(inputs['conv_ln_b'])[:, None, :]], axis=1)
    w_r = np.concatenate([f(inputs['w_router_group']), f(inputs['w_router_expert'])], axis=2)
    b_r = np.concatenate([f(inputs['b_router_group']), f(inputs['b_router_expert'])], axis=1)
    shared = {
        "w_mod": f(inputs['w_mod']), "b_mod": f(inputs['b_mod']), "g_norm1": f(inputs['g_norm1']), "w_in": f(inputs['w_in']),
        "conv_pack": np.ascontiguousarray(conv_pack), "w_out": f(inputs['w_out']), "g_norm2": f(inputs['g_norm2']),
        "w_r": np.ascontiguousarray(w_r), "b_r": np.ascontiguousarray(b_r),
        "w_exp_gate": f(inputs['w_exp_gate']), "w_exp_up": f(inputs['w_exp_up']), "w_exp_down": f(inputs['w_exp_down']),
        "g_final": f(inputs['g_final']).reshape(1, D), "cf32": cf, "cbf": cb, "wtab": wt,
    }
    x = f(inputs['x'])
    c = f(inputs['c'])
    maps = []
    for b in range(n_cores):
        m = dict(shared)
        m["x"] = np.ascontiguousarray(x[b])
        m["c"] = np.ascontiguousarray(c[b:b + 1])
        maps.append(m)
    return maps


def kernel(**inputs):
    if 'nc' not in _CACHE:
        _CACHE['nc'] = build_program(DEPTH, dbg=False)[0]
    nc = _CACHE['nc']
    maps = make_in_maps(inputs, 8)
    res = run_bass_kernel_spmd(nc, maps, core_ids=list(range(8)))
    out = np.stack([np.asarray(res.results[b]["out"], dtype=np.float32) for b in range(8)], axis=0)
    return out
```
